# Optimizing a Trainium2 kernel written in Bass

```python
import math
import jax, jax.numpy as jnp
from jax import lax
import numpy as np

D_MODEL = 2048
BATCH = 2
SEQ = 16384
DEPTH = 2

SGU_GROUPS = 8
SGU_WIDTH = 1024
SGU_GROUP_DIM = SGU_WIDTH // SGU_GROUPS
CHUNK = 128
DIFF_HEADS = 8
DIFF_HEAD_DIM = 64
DIFF_V_DIM = 2 * DIFF_HEAD_DIM
DIFF_QK_WIDTH = DIFF_HEADS * 2 * DIFF_HEAD_DIM
DIFF_WIDTH = DIFF_HEADS * DIFF_V_DIM
Q_BLOCK = 128
N_BRANCHES = 2
IN_COLS = 2 * SGU_WIDTH + 2 * DIFF_QK_WIDTH + DIFF_WIDTH + N_BRANCHES * D_MODEL
N_GROUPS = 4
EXPERTS_PER_GROUP = 4
N_EXPERTS = N_GROUPS * EXPERTS_PER_GROUP
EXPERT_FF = 512
TOP_K_IN_GROUP = 2

RMS_EPS = 1e-6
LN_EPS = 1e-5

kernel_name = "hybrid_sgu_diffattn_hmoe_encoder"


def rmsnorm(x, g):
    xf = x.astype(jnp.float32)
    y = xf * lax.rsqrt(jnp.mean(xf * xf, axis=-1, keepdims=True) + RMS_EPS)
    return (y * g.astype(jnp.float32)).astype(x.dtype)


def layernorm(x, g, b):
    xf = x.astype(jnp.float32)
    mu = jnp.mean(xf, axis=-1, keepdims=True)
    var = jnp.mean(jnp.square(xf - mu), axis=-1, keepdims=True)
    y = (xf - mu) * lax.rsqrt(var + LN_EPS)
    return (y * g.astype(jnp.float32) + b.astype(jnp.float32)).astype(x.dtype)


def alibi_slopes(n_heads):
    return jnp.exp2(-8.0 * jnp.arange(1, n_heads + 1, dtype=jnp.float32) / n_heads)


def spatial_gating(u, v, ln_g, ln_b, w_s, b_s):
    B, S, _ = v.shape
    u = jax.nn.gelu(u)
    v = layernorm(jax.nn.gelu(v), ln_g, ln_b)
    vc = v.reshape(B, S // CHUNK, CHUNK, SGU_GROUPS, SGU_GROUP_DIM)
    mixed = jnp.einsum('gts,bnsgc->bntgc', w_s, vc) + b_s.T[None, None, :, :, None]
    return u * mixed.reshape(B, S, SGU_WIDTH)


def diff_attention(q, k, v, lam, slopes):
    B, S = q.shape[:2]
    nb = S // Q_BLOCK
    scale = DIFF_HEAD_DIM ** -0.5
    qb = q.reshape(B, nb, Q_BLOCK, DIFF_HEADS, 2, DIFF_HEAD_DIM).transpose(1, 0, 2, 3, 4, 5)
    starts = jnp.arange(nb, dtype=jnp.int32) * Q_BLOCK
    k_pos = jnp.arange(S, dtype=jnp.int32)

    def block(args):
        q_blk, start = args
        q_pos = start + jnp.arange(Q_BLOCK, dtype=jnp.int32)
        dist = jnp.abs(q_pos[:, None] - k_pos[None, :]).astype(jnp.float32)
        bias = -slopes[:, None, None] * dist[None]
        s = jnp.einsum('bqhmd,bkhmd->mbhqk', q_blk, k,
                       preferred_element_type=jnp.float32) * scale + bias
        p = jax.nn.softmax(s, axis=-1)
        w = p[0] - lam * p[1]
        return jnp.einsum('bhqk,bkhe->bqhe', w.astype(v.dtype), v)

    out = lax.map(block, (qb, starts))
    return out.transpose(1, 0, 2, 3, 4).reshape(B, S, DIFF_HEADS, DIFF_V_DIM)


def hier_moe(h, rg_w, rg_b, re_w, re_b, w1, w3, w2):
    B, S, D = h.shape
    t = h.reshape(B * S, D)
    g_logits = (t @ rg_w).astype(jnp.float32) + rg_b.astype(jnp.float32)
    g_prob = jax.nn.softmax(g_logits, axis=-1)
    g_w, g_idx = lax.top_k(g_prob, 1)
    e_logits = ((t @ re_w).astype(jnp.float32) + re_b.astype(jnp.float32)
                ).reshape(-1, N_GROUPS, EXPERTS_PER_GROUP)
    g_onehot = jax.nn.one_hot(g_idx[:, 0], N_GROUPS, dtype=jnp.float32)
    e_sel = jnp.einsum('tg,tge->te', g_onehot, e_logits)
    e_prob = jax.nn.softmax(e_sel, axis=-1)
    e_w, e_idx = lax.top_k(e_prob, TOP_K_IN_GROUP)
    e_w = e_w / jnp.sum(e_w, axis=-1, keepdims=True)
    weights = g_w * e_w
    experts = g_idx * EXPERTS_PER_GROUP + e_idx
    combine = jnp.einsum('tk,tke->et', weights,
                         jax.nn.one_hot(experts, N_EXPERTS, dtype=jnp.float32)).astype(t.dtype)

    def body(y, xs):
        w1_e, w3_e, w2_e, c_e = xs
        hid = jax.nn.silu(t @ w1_e) * (t @ w3_e)
        return y + c_e[:, None] * (hid @ w2_e), None

    y, _ = lax.scan(body, jnp.zeros_like(t), (w1, w3, w2, combine))
    return y.reshape(B, S, D)


def setup_inputs(seed: int = 0) -> dict:
    key = jax.random.key(seed)
    ks = jax.random.split(key, 24)
    f32 = jnp.float32

    def nrm(k, shape, scale):
        return jax.random.normal(k, shape, f32) * scale

    L, D = DEPTH, D_MODEL
    return {
        "x": nrm(ks[0], (BATCH, SEQ, D), 1.0),
        "norm1_g": 1.0 + nrm(ks[1], (L, D), 0.02),
        "w_in": nrm(ks[2], (L, D, IN_COLS), D ** -0.5),
        "b_gate": nrm(ks[3], (L, N_BRANCHES, D), 0.02),
        "sgu_ln_g": 1.0 + nrm(ks[4], (L, SGU_WIDTH), 0.02),
        "sgu_ln_b": nrm(ks[5], (L, SGU_WIDTH), 0.02),
        "sgu_w": nrm(ks[6], (L, SGU_GROUPS, CHUNK, CHUNK), CHUNK ** -0.5),
        "sgu_b": 1.0 + nrm(ks[7], (L, SGU_GROUPS, CHUNK), 0.02),
        "lam_q1": nrm(ks[8], (L, DIFF_HEAD_DIM), 0.1),
        "lam_k1": nrm(ks[9], (L, DIFF_HEAD_DIM), 0.1),
        "lam_q2": nrm(ks[10], (L, DIFF_HEAD_DIM), 0.1),
        "lam_k2": nrm(ks[11], (L, DIFF_HEAD_DIM), 0.1),
        "diff_norm_g": 1.0 + nrm(ks[12], (L, DIFF_V_DIM), 0.02),
        "w_proj_a": nrm(ks[13], (L, SGU_WIDTH, D), SGU_WIDTH ** -0.5),
        "w_proj_b": nrm(ks[14], (L, DIFF_WIDTH, D), DIFF_WIDTH ** -0.5),
        "w_out": nrm(ks[15], (L, D, D), D ** -0.5),
        "norm2_g": 1.0 + nrm(ks[16], (L, D), 0.02),
        "router_g_w": nrm(ks[17], (L, D, N_GROUPS), D ** -0.5),
        "router_g_b": nrm(ks[18], (L, N_GROUPS), 0.01),
        "router_e_w": nrm(ks[19], (L, D, N_EXPERTS), D ** -0.5),
        "router_e_b": nrm(ks[20], (L, N_EXPERTS), 0.01),
        "w1": nrm(ks[21], (L, N_EXPERTS, D, EXPERT_FF), D ** -0.5),
        "w3": nrm(ks[22], (L, N_EXPERTS, D, EXPERT_FF), D ** -0.5),
        "w2": nrm(ks[23], (L, N_EXPERTS, EXPERT_FF, D), EXPERT_FF ** -0.5),
        "final_g": 1.0 + nrm(jax.random.fold_in(key, 99), (D,), 0.02),
    }


def reference(x, norm1_g, w_in, b_gate, sgu_ln_g, sgu_ln_b, sgu_w, sgu_b,
              lam_q1, lam_k1, lam_q2, lam_k2, diff_norm_g, w_proj_a, w_proj_b,
              w_out, norm2_g, router_g_w, router_g_b, router_e_w, router_e_b,
              w1, w3, w2, final_g):
    B, S, D = x.shape
    slopes = alibi_slopes(DIFF_HEADS)
    splits = [SGU_WIDTH, 2 * SGU_WIDTH,
              2 * SGU_WIDTH + DIFF_QK_WIDTH,
              2 * SGU_WIDTH + 2 * DIFF_QK_WIDTH,
              2 * SGU_WIDTH + 2 * DIFF_QK_WIDTH + DIFF_WIDTH]
    for l in range(DEPTH):
        h = rmsnorm(x, norm1_g[l])
        z = h @ w_in[l]
        u_a, v_a, q, k, v_b, gate_logits = jnp.split(z, splits, axis=-1)

        a = spatial_gating(u_a, v_a, sgu_ln_g[l], sgu_ln_b[l], sgu_w[l], sgu_b[l])

        lam_init = 0.8 - 0.6 * math.exp(-0.3 * l)
        lam = (jnp.exp(jnp.sum(lam_q1[l].astype(jnp.float32) * lam_k1[l].astype(jnp.float32)))
               - jnp.exp(jnp.sum(lam_q2[l].astype(jnp.float32) * lam_k2[l].astype(jnp.float32)))
               + lam_init)
        qh = q.reshape(B, S, DIFF_HEADS, 2, DIFF_HEAD_DIM)
        kh = k.reshape(B, S, DIFF_HEADS, 2, DIFF_HEAD_DIM)
        vh = v_b.reshape(B, S, DIFF_HEADS, DIFF_V_DIM)
        o = diff_attention(qh, kh, vh, lam, slopes)
        o = rmsnorm(o, diff_norm_g[l]) * (1.0 - lam_init)
        b_out = o.reshape(B, S, DIFF_WIDTH)

        gates = jax.nn.sigmoid(gate_logits.reshape(B, S, N_BRANCHES, D) + b_gate[l])
        merged = gates[:, :, 0] * (a @ w_proj_a[l]) + gates[:, :, 1] * (b_out @ w_proj_b[l])
        x = x + merged @ w_out[l]

        h2 = rmsnorm(x, norm2_g[l])
        x = x + hier_moe(h2, router_g_w[l], router_g_b[l], router_e_w[l], router_e_b[l],
                         w1[l], w3[l], w2[l])
    return rmsnorm(x, final_g)
```

```python
import math
from contextlib import ExitStack

import numpy as np
import ml_dtypes
import concourse.bass as bass
import concourse.mybir as mybir
from concourse.bass_utils import run_bass_kernel_spmd

F32 = mybir.dt.float32
BF16 = mybir.dt.bfloat16
AF = mybir.ActivationFunctionType
ALU = mybir.AluOpType
NPBF = ml_dtypes.bfloat16

D = 2048
NCORE = 8
NT = 4096
SEQ = 16384
NH = 8
RMS_EPS = 1e-6
LN_EPS = 1e-5
NEXP = 16
FF = 512

EPOCH = 16000
NDMASEM = 8
SAME_ENGINE_SYNC = True


class T:
    __slots__ = ("w", "r")

    def __init__(self):
        self.w = None
        self.r = []


class Op:
    __slots__ = ("eng", "fn", "dma", "deps", "signal", "tick", "idx", "dsem", "dval", "prev_dma")

    def __init__(self, eng, fn, dma):
        self.eng = eng
        self.fn = fn
        self.dma = dma
        self.deps = []
        self.signal = False
        self.tick = 0
        self.idx = 0
        self.dsem = None
        self.dval = 0
        self.prev_dma = None


class Sched:
    ENGS = ("pe", "act", "dve", "pool", "sp")

    def __init__(self, nc):
        self.nc = nc
        self.ops = {e: [] for e in self.ENGS}
        self.ndma = {e: 0 for e in self.ENGS}
        self.dma_hist = {e: [] for e in self.ENGS}

    def op(self, eng, fn, reads=(), writes=(), dma=False):
        o = Op(eng, fn, dma)
        o.idx = len(self.ops[eng])
        deps = {}

        def add(d):
            if d is None or d is o:
                return
            if (not d.dma) and d.eng == eng:
                if not o.dma and (eng == "pe" or not SAME_ENGINE_SYNC):
                    return
            if d.dma:
                deps[("dma", id(d))] = d
            else:
                k = ("c", d.eng)
                if k not in deps or deps[k].idx < d.idx:
                    deps[k] = d

        for t in reads:
            add(t.w)
        for t in writes:
            add(t.w)
            for rr in t.r:
                add(rr)
        o.deps = list(deps.values())
        for d in o.deps:
            d.signal = True
        for t in reads:
            t.r.append(o)
            if len(t.r) > 48:
                keep = {}
                for rr in t.r:
                    if rr.dma:
                        keep[id(rr)] = rr
                    elif rr.eng not in keep or keep[rr.eng].idx < rr.idx:
                        keep[rr.eng] = rr
                t.r = list(keep.values())
        for t in writes:
            t.w = o
            t.r = []
        if dma:
            o.signal = True
            j = self.ndma[eng]
            self.ndma[eng] += 1
            hist = self.dma_hist[eng]
            if j >= NDMASEM:
                o.prev_dma = hist[j - NDMASEM]
            hist.append(o)
            o.dval = 16 * (j // NDMASEM + 1)
            o.dsem = j % NDMASEM
        self.ops[eng].append(o)
        return o

    def emit(self, stack):
        nc = self.nc
        nepoch = {}
        for e in self.ENGS:
            c = 0
            for o in self.ops[e]:
                if o.signal and not o.dma:
                    c += 1
                    o.tick = c
            nepoch[e] = (c + EPOCH - 1) // EPOCH
        csem = {e: [stack.enter_context(nc.semaphore(f"c_{e}_{k}")) for k in range(nepoch[e])]
                for e in self.ENGS}
        dsem = {e: ([stack.enter_context(nc.semaphore(f"d_{e}_{k}")) for k in range(NDMASEM)]
                    if self.ndma[e] else []) for e in self.ENGS}

        def semval(d):
            if d.dma:
                return dsem[d.eng][d.dsem], d.dval
            k = (d.tick - 1) // EPOCH
            return csem[d.eng][k], d.tick - k * EPOCH

        block = stack.enter_context(nc.Block())
        reg = {"pe": block.tensor, "act": block.scalar, "dve": block.vector,
               "pool": block.gpsimd, "sp": block.sync}

        def make(e):
            def body(h):
                waited = {}
                for o in self.ops[e]:
                    ws = [semval(d) for d in o.deps]
                    if o.prev_dma is not None:
                        ws.append(semval(o.prev_dma))
                    for s, v in ws:
                        if waited.get(id(s), 0) >= v:
                            continue
                        waited[id(s)] = v
                        h.wait_ge(s, v)
                    ins = o.fn(h)
                    if o.signal:
                        s, v = semval(o)
                        ins.then_inc(s, 16 if o.dma else 1)
                for o in self.dma_hist[e][-NDMASEM:]:
                    s, v = semval(o)
                    if waited.get(id(s), 0) < v:
                        waited[id(s)] = v
                        h.wait_ge(s, v)
            return body

        for e in self.ENGS:
            if self.ops[e]:
                reg[e](make(e))


class Rot:
    def __init__(self, st, nc, name, shape, dt, n, psum=False):
        self.bufs = []
        for i in range(n):
            if psum:
                b = st.enter_context(nc.psum_tensor(f"{name}{i}", shape, dt))
            else:
                b = st.enter_context(nc.sbuf_tensor(f"{name}{i}", shape, dt))
            self.bufs.append((b, T()))
        self.i = 0

    def next(self):
        r = self.bufs[self.i % len(self.bufs)]
        self.i += 1
        return r


def _mm_group(S, out_ap, pairs, reads, tout):
    n = len(pairs)
    for k, (l, r) in enumerate(pairs):
        S.op("pe", (lambda h, l=l, r=r, k=k: h.matmul(out_ap, lhsT=l, rhs=r, start=(k == 0), stop=(k == n - 1))),
             reads=reads, writes=[tout])


def build_A():
    nc = bass.Bass("TRN2", target_bir_lowering=False)
    dr = lambda n, s, d, k: nc.dram_tensor(n, s, d, kind=k).ap()
    x = dr("x", [NT, D], F32, "ExternalInput")
    g1 = dr("g1", [1, D], F32, "ExternalInput")
    wf = dr("wf", [72, 128, 16 * 128], F32, "ExternalInput")
    wt = dr("wt", [2, 128, 16 * 1024], F32, "ExternalInput")
    bg = dr("bg", [128, 32], F32, "ExternalInput")
    lng = dr("lng", [1, 1024], F32, "ExternalInput")
    lnb = dr("lnb", [1, 1024], F32, "ExternalInput")
    wst = dr("wst", [128, 8 * 128], F32, "ExternalInput")
    sb_ = dr("sgub", [1, 1024], F32, "ExternalInput")
    wa = dr("wa", [16, 128, 8 * 128], F32, "ExternalInput")
    ident_d = dr("ident", [128, 128], F32, "ExternalInput")
    QT = dr("QT", [1024, NT], BF16, "ExternalOutput")
    KT = dr("KT", [1024, NT], BF16, "ExternalOutput")
    VV = dr("VV", [NT, 1024], BF16, "ExternalOutput")
    M0T = dr("M0T", [D, NT], F32, "ExternalOutput")
    G1T = dr("G1T", [D, NT], F32, "ExternalOutput")
    S = Sched(nc)
    HALF = 1024
    with ExitStack() as st:
        sbt = lambda n, s, d: st.enter_context(nc.sbuf_tensor(n, s, d))
        ident = sbt("identb", [128, 128], BF16); Tid = T()
        g1bc = sbt("g1bc", [128, D], F32); Tg1 = T()
        lngbc = sbt("lngbc", [128, 1024], F32); lnbbc = sbt("lnbbc", [128, 1024], F32); Tln = T()
        bsb = sbt("bsb", [128, 1024], F32); Tbsb = T()
        wsT = sbt("wsT", [128, 1024], BF16); Tws = T()
        bgt = sbt("bgt", [128, 32], F32); Tbg = T()
        epsr = sbt("epsr", [128, 1], F32); epsl = sbt("epsl", [128, 1], F32); Teps = T()
        hT = sbt("hT", [128, 16, HALF], BF16); ThT = [T() for _ in range(2)]
        guT = sbt("guT", [128, 8, HALF], BF16); Tgu = [T() for _ in range(2)]
        aT = sbt("aT", [128, 8, HALF], BF16); TaT = [T() for _ in range(2)]
        wtb = sbt("wtb", [128, 16, 1024], BF16); Twt = T()
        xr = Rot(st, nc, "xin", [128, D], F32, 2)
        h16 = Rot(st, nc, "h16", [128, D], BF16, 2)
        junk = Rot(st, nc, "junk", [128, D], BF16, 1)
        sm = Rot(st, nc, "sm", [128, 8], F32, 4)
        wfb = Rot(st, nc, "wfb", [128, 16, 128], BF16, 3)
        wab = Rot(st, nc, "wab", [128, 8, 128], BF16, 2)
        stg16 = Rot(st, nc, "stg16", [128, 1024], BF16, 3)
        stg32 = Rot(st, nc, "stg32", [128, 512], F32, 4)
        gv = Rot(st, nc, "gv", [128, 1024], F32, 2)
        vln = Rot(st, nc, "vln", [128, 1024], BF16, 2)
        tmp32 = Rot(st, nc, "tmp32", [128, 1024], F32, 2)
        pst = Rot(st, nc, "pst", [128, 512], BF16, 2, psum=True)
        psm = Rot(st, nc, "psm", [128, 512], F32, 4, psum=True)
        psw = Rot(st, nc, "psw", [128, 1024], F32, 1, psum=True)

        S.op("sp", lambda h: h.dma_start(out=g1bc[:], in_=g1.partition_broadcast(128)), writes=[Tg1], dma=True)
        S.op("sp", lambda h: h.dma_start(out=lngbc[:], in_=lng.partition_broadcast(128)), writes=[Tln], dma=True)
        S.op("sp", lambda h: h.dma_start(out=lnbbc[:], in_=lnb.partition_broadcast(128)), writes=[Tln], dma=True)
        S.op("sp", lambda h: h.dma_start(out=bsb[:], in_=sb_.partition_broadcast(128)), writes=[Tbsb], dma=True)
        S.op("sp", lambda h: h.dma_start(out=bgt[:], in_=bg), writes=[Tbg], dma=True)
        S.op("pool", lambda h: h.dma_start(out=ident[:], in_=ident_d), writes=[Tid], dma=True)
        S.op("pool", lambda h: h.dma_start(out=wsT[:], in_=wst), writes=[Tws], dma=True)
        S.op("dve", lambda h: h.memset(epsr[:], RMS_EPS), writes=[Teps])
        S.op("dve", lambda h: h.memset(epsl[:], LN_EPS), writes=[Teps])

        def do_half(half):
            t0 = half * HALF
            for tt in range(8):
                tg = tt // 4
                xb, Tx = xr.next()
                rows = slice(t0 + tt * 128, t0 + (tt + 1) * 128)
                S.op("sp", lambda h, xb=xb, rows=rows: h.dma_start(out=xb[:], in_=x[rows, :]), writes=[Tx], dma=True)
                jb, Tj = junk.next(); smb, Tsm = sm.next()
                S.op("act", lambda h, xb=xb, jb=jb, smb=smb: h.activation(out=jb[:], in_=xb[:], func=AF.Square, accum_out=smb[:, 0:1]),
                     reads=[Tx], writes=[Tj, Tsm])
                S.op("act", lambda h, smb=smb: h.activation(out=smb[:, 1:2], in_=smb[:, 0:1], func=AF.Sqrt, bias=epsr[:, 0:1], scale=1.0 / D),
                     reads=[Tsm, Teps], writes=[Tsm])
                S.op("dve", lambda h, smb=smb: h.reciprocal(out=smb[:, 2:3], in_=smb[:, 1:2]), reads=[Tsm], writes=[Tsm])
                hb, Th = h16.next()
                S.op("dve", lambda h, hb=hb, xb=xb, smb=smb: h.scalar_tensor_tensor(out=hb[:], in0=xb[:], scalar=smb[:, 2:3], in1=g1bc[:], op0=ALU.mult, op1=ALU.mult),
                     reads=[Tx, Tsm, Tg1], writes=[Th])
                for c4 in range(4):
                    pb, Tp = pst.next()
                    for k in range(4):
                        c = c4 * 4 + k
                        S.op("pe", lambda h, pb=pb, hb=hb, c=c, k=k: h.transpose(pb[:, k * 128:(k + 1) * 128], hb[:, c * 128:(c + 1) * 128], ident[:]),
                             reads=[Th, Tid], writes=[Tp])
                    eng = "act" if c4 % 2 == 0 else "dve"
                    dst = hT[:, c4 * 4:(c4 + 1) * 4, tt * 128:(tt + 1) * 128]
                    src = pb[:].rearrange("p (k t) -> p k t", k=4)
                    if eng == "act":
                        S.op("act", lambda h, dst=dst, src=src: h.copy(out=dst, in_=src), reads=[Tp], writes=[ThT[tg]])
                    else:
                        S.op("dve", lambda h, dst=dst, src=src: h.tensor_copy(out=dst, in_=src), reads=[Tp], writes=[ThT[tg]])

            def fm_group(cg, evac):
                wb, Tw = wfb.next()
                S.op("pool", lambda h, wb=wb, cg=cg: h.dma_start(out=wb[:].rearrange("p c n -> p (c n)"), in_=wf[cg]), writes=[Tw], dma=True)
                for tg in range(2):
                    pb, Tp = psm.next()
                    _mm_group(S, pb[:], [(wb[:, c, :], hT[:, c, tg * 512:(tg + 1) * 512]) for c in range(16)], [Tw, ThT[tg]], Tp)
                    evac(tg, pb, Tp)

            for cg in range(8):
                def ev(tg, pb, Tp, cg=cg):
                    S.op("act", lambda h: h.activation(out=guT[:, cg, tg * 512:(tg + 1) * 512], in_=pb[:], func=AF.Gelu_apprx_tanh),
                         reads=[Tp], writes=[Tgu[tg]])
                fm_group(cg, ev)
            for q in range(4):
                S.op("pool", lambda h, q=q: h.dma_start(out=wtb[:, q * 4:(q + 1) * 4, :].rearrange("p c n -> p (c n)"), in_=wt[0, :, q * 4096:(q + 1) * 4096]),
                     writes=[Twt], dma=True)
            for tt in range(8):
                tg = tt // 4
                ts = slice(tt * 128, (tt + 1) * 128)
                gb, Tgv = gv.next(); smb, Tsm = sm.next()
                pw, Tpw = psw.next()
                for hh in range(2):
                    _mm_group(S, pw[:, hh * 512:(hh + 1) * 512], [(hT[:, c, ts], wtb[:, c, hh * 512:(hh + 1) * 512]) for c in range(16)], [Twt, ThT[tg]], Tpw)
                S.op("act", lambda h, gb=gb, pw=pw, smb=smb: h.activation(out=gb[:], in_=pw[:], func=AF.Gelu_apprx_tanh, accum_out=smb[:, 0:1]),
                     reads=[Tpw], writes=[Tgv, Tsm])
                jb, Tj = junk.next()
                S.op("act", lambda h, gb=gb, jb=jb, smb=smb: h.activation(out=jb[:, 0:1024], in_=gb[:], func=AF.Square, accum_out=smb[:, 1:2]),
                     reads=[Tgv], writes=[Tj, Tsm])
                S.op("dve", lambda h, smb=smb: h.tensor_scalar(out=smb[:, 2:3], in0=smb[:, 0:1], scalar1=1.0 / 1024, scalar2=None, op0=ALU.mult), reads=[Tsm], writes=[Tsm])
                S.op("dve", lambda h, smb=smb: h.tensor_tensor(out=smb[:, 3:4], in0=smb[:, 2:3], in1=smb[:, 2:3], op=ALU.mult), reads=[Tsm], writes=[Tsm])
                S.op("dve", lambda h, smb=smb: h.scalar_tensor_tensor(out=smb[:, 4:5], in0=smb[:, 1:2], scalar=1.0 / 1024, in1=smb[:, 3:4], op0=ALU.mult, op1=ALU.subtract),
                     reads=[Tsm], writes=[Tsm])
                S.op("act", lambda h, smb=smb: h.activation(out=smb[:, 5:6], in_=smb[:, 4:5], func=AF.Sqrt, bias=epsl[:, 0:1], scale=1.0), reads=[Tsm, Teps], writes=[Tsm])
                S.op("dve", lambda h, smb=smb: h.reciprocal(out=smb[:, 6:7], in_=smb[:, 5:6]), reads=[Tsm], writes=[Tsm])
                tb, Tt = tmp32.next()
                S.op("dve", lambda h, tb=tb, gb=gb, smb=smb: h.tensor_scalar(out=tb[:], in0=gb[:], scalar1=smb[:, 2:3], scalar2=smb[:, 6:7], op0=ALU.subtract, op1=ALU.mult),
                     reads=[Tgv, Tsm], writes=[Tt])
                S.op("pool", lambda h, tb=tb: h.tensor_tensor(out=tb[:], in0=tb[:], in1=lngbc[:], op=ALU.mult), reads=[Tt, Tln], writes=[Tt])
                vb, Tv = vln.next()
                S.op("dve", lambda h, tb=tb, vb=vb: h.tensor_tensor(out=vb[:], in0=tb[:], in1=lnbbc[:], op=ALU.add), reads=[Tt, Tln], writes=[Tv])
                pw2, Tpw2 = psw.next()
                for g in range(8):
                    S.op("pe", lambda h, pw2=pw2, vb=vb, g=g: h.matmul(pw2[:, g * 128:(g + 1) * 128], lhsT=vb[:, g * 128:(g + 1) * 128], rhs=wsT[:, g * 128:(g + 1) * 128], start=True, stop=True),
                         reads=[Tv, Tws], writes=[Tpw2])
                tb2, Tt2 = tmp32.next()
                S.op("dve", lambda h, tb2=tb2, pw2=pw2: h.tensor_tensor(out=tb2[:], in0=pw2[:], in1=bsb[:], op=ALU.add), reads=[Tpw2, Tbsb], writes=[Tt2])
                S.op("pool", lambda h, tb2=tb2, ts=ts: h.tensor_tensor(out=aT[:, :, ts], in0=tb2[:].rearrange("p (g t) -> p g t", g=8), in1=guT[:, :, ts], op=ALU.mult),
                     reads=[Tt2, Tgu[tg]], writes=[TaT[tg]])
            for which, base_cg, dst, scl in (("q", 16, QT, 0.125), ("k", 24, KT, 1.0)):
                for hh in range(8):
                    def ev(tg, pb, Tp, hh=hh, dst=dst, scl=scl):
                        sb2, Ts = stg16.next()
                        S.op("act", lambda h: h.activation(out=sb2[:, 0:512], in_=pb[:], func=AF.Copy, scale=scl), reads=[Tp], writes=[Ts])
                        S.op("sp", lambda h: h.dma_start(out=dst[hh * 128:(hh + 1) * 128, t0 + tg * 512:t0 + (tg + 1) * 512], in_=sb2[:, 0:512]), reads=[Ts], dma=True)
                    fm_group(base_cg + hh, ev)
            for q in range(4):
                S.op("pool", lambda h, q=q: h.dma_start(out=wtb[:, q * 4:(q + 1) * 4, :].rearrange("p c n -> p (c n)"), in_=wt[1, :, q * 4096:(q + 1) * 4096]),
                     writes=[Twt], dma=True)
            for tt in range(8):
                tg = tt // 4
                ts = slice(tt * 128, (tt + 1) * 128)
                pw, Tpw = psw.next()
                for hh in range(2):
                    _mm_group(S, pw[:, hh * 512:(hh + 1) * 512], [(hT[:, c, ts], wtb[:, c, hh * 512:(hh + 1) * 512]) for c in range(16)], [Twt, ThT[tg]], Tpw)
                sb2, Ts = stg16.next()
                S.op("dve", lambda h, sb2=sb2, pw=pw: h.tensor_copy(out=sb2[:], in_=pw[:]), reads=[Tpw], writes=[Ts])
                S.op("sp", lambda h, sb2=sb2, tt=tt: h.dma_start(out=VV[t0 + tt * 128:t0 + (tt + 1) * 128, :], in_=sb2[:]), reads=[Ts], dma=True)
            for dc in range(16):
                def ev(tg, pb, Tp, dc=dc):
                    sb3, Ts = stg32.next()
                    S.op("act", lambda h: h.activation(out=sb3[:], in_=pb[:], func=AF.Sigmoid, bias=bgt[:, 16 + dc:17 + dc], scale=1.0), reads=[Tp, Tbg], writes=[Ts])
                    S.op("sp", lambda h: h.dma_start(out=G1T[dc * 128:(dc + 1) * 128, t0 + tg * 512:t0 + (tg + 1) * 512], in_=sb3[:]), reads=[Ts], dma=True)
                fm_group(56 + dc, ev)
            for dc in range(16):
                wab_, Twa = wab.next()
                S.op("pool", lambda h, wab_=wab_, dc=dc: h.dma_start(out=wab_[:].rearrange("p c n -> p (c n)"), in_=wa[dc]), writes=[Twa], dma=True)
                def ev(tg, pb, Tp, dc=dc, wab_=wab_, Twa=Twa):
                    sb3, Ts = stg32.next()
                    S.op("act", lambda h: h.activation(out=sb3[:], in_=pb[:], func=AF.Sigmoid, bias=bgt[:, dc:dc + 1], scale=1.0), reads=[Tp, Tbg], writes=[Ts])
                    pb2, Tp2 = psm.next()
                    _mm_group(S, pb2[:], [(wab_[:, c, :], aT[:, c, tg * 512:(tg + 1) * 512]) for c in range(8)], [Twa, TaT[tg]], Tp2)
                    S.op("dve", lambda h: h.tensor_tensor(out=sb3[:], in0=pb2[:], in1=sb3[:], op=ALU.mult), reads=[Tp2, Ts], writes=[Ts])
                    S.op("sp", lambda h: h.dma_start(out=M0T[dc * 128:(dc + 1) * 128, t0 + tg * 512:t0 + (tg + 1) * 512], in_=sb3[:]), reads=[Ts], dma=True)
                fm_group(40 + dc, ev)
        for half in range(4):
            do_half(half)
        S.emit(st)
    return nc


def _tile_km(w, ncols):
    K, N = w.shape
    return np.ascontiguousarray(w.reshape(K // 128, 128, N // ncols, ncols).transpose(2, 1, 0, 3)).reshape(N // ncols, 128, (K // 128) * ncols)


def prep_A(l, inp):
    w_in = np.asarray(inp["w_in"][l])
    wf = _tile_km(w_in, 128)
    wt = np.stack([_tile_km(w_in[:, 1024:2048], 1024)[0], _tile_km(w_in[:, 4096:5120], 1024)[0]])
    bg = np.ascontiguousarray(np.asarray(inp["b_gate"][l]).reshape(32, 128).T)
    wst = np.ascontiguousarray(np.asarray(inp["sgu_w"][l]).transpose(2, 0, 1)).reshape(128, 1024)
    return {
        "g1": np.asarray(inp["norm1_g"][l]).reshape(1, D),
        "wf": wf, "wt": wt, "bg": bg,
        "lng": np.asarray(inp["sgu_ln_g"][l]).reshape(1, 1024),
        "lnb": np.asarray(inp["sgu_ln_b"][l]).reshape(1, 1024),
        "wst": wst,
        "sgub": np.asarray(inp["sgu_b"][l]).reshape(1, 1024),
        "wa": _tile_km(np.asarray(inp["w_proj_a"][l]), 128),
        "ident": np.eye(128, dtype=np.float32),
    }


def build_B(lam_init, heads=NH, nqs=8):
    nc = bass.Bass("TRN2", target_bir_lowering=False)
    dr = lambda n, s, d, k: nc.dram_tensor(n, s, d, kind=k).ap()
    QA = dr("QA", [NH, 2, 66, NT], BF16, "ExternalInput")
    KA = dr("KA", [NH, 2, 66, SEQ], BF16, "ExternalInput")
    VH = dr("VH", [NH, 128, 128 * 129], BF16, "ExternalInput")
    btab_d = dr("btab", [128, NH * 8 * 128], F32, "ExternalInput")
    dmat_d = dr("dmat", [128, 4 * 1024], F32, "ExternalInput")
    negi_d = dr("negi", [128, 1024], F32, "ExternalInput")
    lamv = dr("lamv", [4, 64], F32, "ExternalInput")
    gdn = dr("gdn", [1, 128], F32, "ExternalInput")
    ident_d = dr("ident", [128, 128], F32, "ExternalInput")
    BOT = dr("BOT", [1024, NT], BF16, "ExternalOutput")
    slopes = [2.0 ** (-(i + 1)) for i in range(NH)]
    S = Sched(nc)
    with ExitStack() as st:
        sbt = lambda n, s, d: st.enter_context(nc.sbuf_tensor(n, s, d))
        ident = sbt("identb", [128, 128], BF16); Tid = T()
        btab = sbt("btabs", [128, NH * 8 * 128], F32); Tbt = T()
        dmat = sbt("dmats", [128, 4 * 1024], F32); Tdm = T()
        negi = sbt("negis", [128, 1024], F32); Tng = T()
        lv = sbt("lv", [128, 4 * 64], F32); lsm = sbt("lsm", [128, 8], F32); Tlam = T()
        gbc = sbt("gbc", [128, 128], F32); Tg = T()
        eps = sbt("epsb", [128, 1], F32)
        K1 = sbt("K1", [66, SEQ], BF16); K2 = sbt("K2", [66, SEQ], BF16); TK = [T(), T()]
        Vs = sbt("Vs", [128, 128 * 129], BF16); TV = [T(), T()]
        Q1 = sbt("Q1", [66, NT], BF16); Q2 = sbt("Q2", [66, NT], BF16); TQ = T()
        PTr = Rot(st, nc, "PT", [128, 1024], BF16, 3)
        psS = Rot(st, nc, "psS", [128, 1024], F32, 2, psum=True)
        accp = st.enter_context(nc.psum_tensor("accp", [128, 3 * 512], F32)); Tacc = T()
        pst = st.enter_context(nc.psum_tensor("pstB", [128, 512], BF16)); Tpst = T()
        esm = Rot(st, nc, "esm", [128, 8], F32, 4)
        t2r = Rot(st, nc, "t2r", [128, 128], F32, 2)
        o32 = Rot(st, nc, "o32", [128, 128], F32, 2)
        ob16 = Rot(st, nc, "ob16", [128, 128], BF16, 2)
        junk = Rot(st, nc, "junkb", [128, 128], F32, 1)
        stg = Rot(st, nc, "stgb", [128, 512], BF16, 2)

        S.op("sp", lambda h: h.dma_start(out=btab[:], in_=btab_d), writes=[Tbt], dma=True)
        S.op("sp", lambda h: h.dma_start(out=dmat[:], in_=dmat_d), writes=[Tdm], dma=True)
        S.op("sp", lambda h: h.dma_start(out=negi[:], in_=negi_d), writes=[Tng], dma=True)
        S.op("pool", lambda h: h.dma_start(out=ident[:], in_=ident_d), writes=[Tid], dma=True)
        for i in range(4):
            S.op("sp", lambda h, i=i: h.dma_start(out=lv[:, i * 64:(i + 1) * 64], in_=lamv[i:i + 1, :].partition_broadcast(128)), writes=[Tlam], dma=True)
        S.op("sp", lambda h: h.dma_start(out=gbc[:], in_=gdn.partition_broadcast(128)), writes=[Tg], dma=True)
        S.op("dve", lambda h: h.memset(eps[:], RMS_EPS), writes=[Tg])
        S.op("dve", lambda h: h.tensor_scalar(out=gbc[:], in0=gbc[:], scalar1=1.0 - lam_init, scalar2=None, op0=ALU.mult), reads=[Tg], writes=[Tg])
        S.op("dve", lambda h: h.tensor_tensor(out=lv[:, 0:64], in0=lv[:, 0:64], in1=lv[:, 64:128], op=ALU.mult), reads=[Tlam], writes=[Tlam])
        S.op("dve", lambda h: h.tensor_tensor(out=lv[:, 128:192], in0=lv[:, 128:192], in1=lv[:, 192:256], op=ALU.mult), reads=[Tlam], writes=[Tlam])
        S.op("dve", lambda h: h.reduce_sum(out=lsm[:, 0:1], in_=lv[:, 0:64], axis=mybir.AxisListType.X), reads=[Tlam], writes=[Tlam])
        S.op("dve", lambda h: h.reduce_sum(out=lsm[:, 1:2], in_=lv[:, 128:192], axis=mybir.AxisListType.X), reads=[Tlam], writes=[Tlam])
        S.op("act", lambda h: h.activation(out=lsm[:, 2:4], in_=lsm[:, 0:2], func=AF.Exp), reads=[Tlam], writes=[Tlam])
        S.op("dve", lambda h: h.tensor_tensor(out=lsm[:, 4:5], in0=lsm[:, 2:3], in1=lsm[:, 3:4], op=ALU.subtract), reads=[Tlam], writes=[Tlam])
        S.op("dve", lambda h: h.tensor_scalar(out=lsm[:, 5:6], in0=lsm[:, 4:5], scalar1=lam_init, scalar2=None, op0=ALU.add), reads=[Tlam], writes=[Tlam])
        lam_ap = lsm[:, 5:6]

        def acc_ap(a, lo, hi):
            return accp[:, (a // 3) * 512 + (a % 3) * 129 + lo:(a // 3) * 512 + (a % 3) * 129 + hi]

        def head(hd):
            m = slopes[hd]
            S.op("sp", lambda h: h.dma_start(out=Q1[:], in_=QA[hd, 0]), writes=[TQ], dma=True)
            S.op("sp", lambda h: h.dma_start(out=Q2[:], in_=QA[hd, 1]), writes=[TQ], dma=True)
            for hf in range(2):
                ks = slice(hf * 8192, (hf + 1) * 8192)
                S.op("sp", lambda h, ks=ks: h.dma_start(out=K1[:, ks], in_=KA[hd, 0, :, ks]), writes=[TK[hf]], dma=True)
                S.op("sp", lambda h, ks=ks: h.dma_start(out=K2[:, ks], in_=KA[hd, 1, :, ks]), writes=[TK[hf]], dma=True)
                vs = slice(hf * 64 * 129, (hf + 1) * 64 * 129)
                S.op("sp", lambda h, vs=vs: h.dma_start(out=Vs[:, vs], in_=VH[hd, :, vs]), writes=[TV[hf]], dma=True)
            def do_qs(qs):
                qsl = slice(qs * 512, (qs + 1) * 512)
                def front(r):
                    hf = r // 64
                    ksl = slice(r * 128, (r + 1) * 128)
                    pS, TpS = psS.next()
                    S.op("pe", lambda h: h.matmul(pS[:, 0:512], lhsT=K1[:, ksl], rhs=Q1[:, qsl], start=True, stop=True), reads=[TK[hf], TQ], writes=[TpS])
                    S.op("pe", lambda h: h.matmul(pS[:, 512:1024], lhsT=K2[:, ksl], rhs=Q2[:, qsl], start=True, stop=True), reads=[TK[hf], TQ], writes=[TpS])
                    if r < 32:
                        dk = r - 4 * qs
                        if 0 <= dk < 4:
                            S.op("dve", lambda h: h.scalar_tensor_tensor(out=pS[:], in0=dmat[:, dk * 1024:(dk + 1) * 1024], scalar=-m, in1=pS[:], op0=ALU.mult, op1=ALU.add),
                                 reads=[TpS, Tdm], writes=[TpS])
                        else:
                            sg = m if dk < 0 else -m
                            S.op("dve", lambda h: h.scalar_tensor_tensor(out=pS[:], in0=negi[:], scalar=sg, in1=pS[:], op0=ALU.mult, op1=ALU.add),
                                 reads=[TpS, Tng], writes=[TpS])
                    pt, Tpt = PTr.next()
                    col = (hd * 8 + qs) * 128 + r
                    S.op("act", lambda h: h.activation(out=pt[:], in_=pS[:], func=AF.Exp, bias=btab[:, col:col + 1], scale=1.0),
                         reads=[TpS, Tbt], writes=[Tpt])
                    return pt, Tpt

                def back(r, pt, Tpt):
                    hf = r // 64
                    for a in range(8):
                        S.op("pe", lambda h, a=a: h.matmul(acc_ap(a, 0, 129), lhsT=pt[:, a * 128:(a + 1) * 128], rhs=Vs[:, r * 129:(r + 1) * 129], start=(r == 0 and a % 3 == 0), stop=(r == 127), skip_group_check=True),
                             reads=[Tpt, TV[hf]], writes=[Tacc])

                cur = front(0)
                for r in range(128):
                    nxt = front(r + 1) if r + 1 < 128 else None
                    back(r, *cur)
                    cur = nxt
                sb2, Ts = stg.next()
                for qt in range(4):
                    sm, Tsm = esm.next()
                    S.op("dve", lambda h, sm=sm, qt=qt: h.reciprocal(out=sm[:, 0:1], in_=acc_ap(qt, 128, 129)), reads=[Tacc], writes=[Tsm])
                    S.op("dve", lambda h, sm=sm, qt=qt: h.reciprocal(out=sm[:, 1:2], in_=acc_ap(4 + qt, 128, 129)), reads=[Tacc], writes=[Tsm])
                    S.op("dve", lambda h, sm=sm: h.tensor_tensor(out=sm[:, 2:3], in0=sm[:, 1:2], in1=lam_ap, op=ALU.mult), reads=[Tsm, Tlam], writes=[Tsm])
                    t2, Tt2 = t2r.next()
                    S.op("dve", lambda h, sm=sm, t2=t2, qt=qt: h.tensor_scalar(out=t2[:], in0=acc_ap(4 + qt, 0, 128), scalar1=sm[:, 2:3], scalar2=None, op0=ALU.mult), reads=[Tacc, Tsm], writes=[Tt2])
                    ob, To = o32.next()
                    S.op("dve", lambda h, sm=sm, t2=t2, ob=ob, qt=qt: h.scalar_tensor_tensor(out=ob[:], in0=acc_ap(qt, 0, 128), scalar=sm[:, 0:1], in1=t2[:], op0=ALU.mult, op1=ALU.subtract),
                         reads=[Tacc, Tsm, Tt2], writes=[To])
                    jb, Tj = junk.next()
                    S.op("act", lambda h, jb=jb, ob=ob, sm=sm: h.activation(out=jb[:], in_=ob[:], func=AF.Square, accum_out=sm[:, 3:4]), reads=[To], writes=[Tj, Tsm])
                    S.op("act", lambda h, sm=sm: h.activation(out=sm[:, 4:5], in_=sm[:, 3:4], func=AF.Sqrt, bias=eps[:, 0:1], scale=1.0 / 128), reads=[Tsm, Tg], writes=[Tsm])
                    S.op("dve", lambda h, sm=sm: h.reciprocal(out=sm[:, 5:6], in_=sm[:, 4:5]), reads=[Tsm], writes=[Tsm])
                    o16, To16 = ob16.next()
                    S.op("dve", lambda h, o16=o16, ob=ob, sm=sm: h.scalar_tensor_tensor(out=o16[:], in0=ob[:], scalar=sm[:, 5:6], in1=gbc[:], op0=ALU.mult, op1=ALU.mult),
                         reads=[To, Tsm, Tg], writes=[To16])
                    S.op("pe", lambda h, o16=o16, qt=qt: h.transpose(pst[:, qt * 128:(qt + 1) * 128], o16[:], ident[:]), reads=[To16, Tid], writes=[Tpst])
                S.op("dve", lambda h, sb2=sb2: h.tensor_copy(out=sb2[:], in_=pst[:]), reads=[Tpst], writes=[Ts])
                S.op("sp", lambda h, sb2=sb2: h.dma_start(out=BOT[hd * 128:(hd + 1) * 128, qsl], in_=sb2[:]), reads=[Ts], dma=True)

            for qs in range(nqs):
                do_qs(qs)

        for hd in range(heads):
            head(hd)
        S.emit(st)
    return nc


def prep_B(l, inp, A_out, c):
    b = c // 4
    c4 = c % 4
    cores = [b * 4 + j for j in range(4)]
    KTall = np.concatenate([np.asarray(A_out[j]["KT"]) for j in cores], axis=1)
    VVall = np.concatenate([np.asarray(A_out[j]["VV"]) for j in cores], axis=0)
    own = list(range(32 * c4, 32 * c4 + 32))
    order = own + [kb for kb in range(128) if kb not in own]
    order = np.asarray(order)
    sig = np.where(order < 32 * c4, 1.0, -1.0)
    sig[:32] = 0.0
    KTl = KTall.reshape(NH, 2, 64, 128, 128)[:, :, :, order, :].reshape(NH, 2, 64, SEQ)
    KAa = np.zeros((NH, 2, 66, SEQ), dtype=NPBF)
    KAa[:, :, :64] = KTl
    KAa[:, :, 64:66] = np.repeat(sig, 128).astype(NPBF)[None, None, None, :]
    QT = np.asarray(A_out[c]["QT"]).reshape(NH, 2, 64, NT)
    QAa = np.zeros((NH, 2, 66, NT), dtype=NPBF)
    QAa[:, :, :64] = QT
    i = np.arange(NT) % 512
    ihi = (i // 2) * 2
    ilo = i % 2
    for h in range(NH):
        m = 2.0 ** (-(h + 1))
        QAa[h, :, 64] = (-m * ihi).astype(NPBF)
        QAa[h, :, 65] = (-m * ilo).astype(NPBF)
    Vl = VVall.reshape(128, 128, NH, 128)[order]
    VHa = np.ones((NH, 128, 128, 129), dtype=NPBF)
    VHa[:, :, :, :128] = Vl.transpose(2, 1, 0, 3)
    p = np.arange(128, dtype=np.float64)
    bt = np.zeros((128, NH, 8, 128), dtype=np.float32)
    for qs in range(8):
        T0 = 4096 * c4 + 512 * qs
        rel = (order[None, :] * 128.0 + p[:, None]) - T0
        sgn = np.where(rel < 0, 1.0, -1.0)
        val = sgn * rel
        val[:, 4 * qs:4 * qs + 4] = 0.0
        for h in range(NH):
            bt[:, h, qs, :] = (2.0 ** (-(h + 1))) * val
    ii = np.arange(512, dtype=np.float32)
    dm = np.zeros((128, 4, 2, 512), dtype=np.float32)
    for dk in range(4):
        dm[:, dk, :, :] = np.abs(ii[None, :] - p[:, None] - 128 * dk)[:, None, :]
    negi = np.broadcast_to(np.tile(-ii, 2)[None, :], (128, 1024)).astype(np.float32)
    lamv = np.stack([np.asarray(inp[k][l]) for k in ("lam_q1", "lam_k1", "lam_q2", "lam_k2")]).astype(np.float32)
    return {"QA": QAa, "KA": KAa, "VH": VHa.reshape(NH, 128, 128 * 129), "btab": bt.reshape(128, -1),
            "dmat": dm.reshape(128, -1), "negi": np.ascontiguousarray(negi), "lamv": lamv,
            "gdn": np.asarray(inp["diff_norm_g"][l]).reshape(1, 128), "ident": np.eye(128, dtype=np.float32)}


def build_C(final, ngroups=8, nexp=NEXP, upto=9):
    nc = bass.Bass("TRN2", target_bir_lowering=False)
    dr = lambda n, s, d, k: nc.dram_tensor(n, s, d, kind=k).ap()
    x = dr("x", [NT, D], F32, "ExternalInput")
    M0T = dr("M0T", [D, NT], F32, "ExternalInput")
    G1T = dr("G1T", [D, NT], F32, "ExternalInput")
    BOT = dr("BOT", [1024, NT], BF16, "ExternalInput")
    wbd = dr("wb", [16, 128, 8 * 128], F32, "ExternalInput")
    wod = dr("wo", [4, 128, 16 * 512], F32, "ExternalInput")
    g2 = dr("g2", [1, D], F32, "ExternalInput")
    fg = dr("fg", [1, D], F32, "ExternalInput")
    wrd = dr("wr", [128, 16 * 20], F32, "ExternalInput")
    brd = dr("br", [1, 20], F32, "ExternalInput")
    w1d = dr("w1t", [NEXP, 128, 16 * 512], F32, "ExternalInput")
    w3d = dr("w3t", [NEXP, 128, 16 * 512], F32, "ExternalInput")
    w2d = dr("w2t", [NEXP, 128, 4 * 2048], F32, "ExternalInput")
    ident_d = dr("ident", [128, 128], F32, "ExternalInput")
    XO = dr("XO", [NT, D], F32, "ExternalOutput")
    S = Sched(nc)
    AX = mybir.AxisListType.X
    with ExitStack() as st:
        sbt = lambda n, s, d: st.enter_context(nc.sbuf_tensor(n, s, d))
        ident = sbt("ident32", [128, 128], F32); Tid = T()
        g2bc = sbt("g2bc", [128, D], F32); fgbc = sbt("fgbc", [128, D], F32); Tg = T()
        wr = sbt("wrs", [128, 16, 20], F32); brbc = sbt("brbc", [128, 20], F32); Twr = T()
        eps = sbt("epsc", [128, 1], F32)
        x1 = [sbt(f"x1_{i}", [128, D], F32) for i in range(4)]; Tx1 = [T() for _ in range(4)]
        cw = [sbt(f"cw_{i}", [128, 16], F32) for i in range(4)]; Tcw = [T() for _ in range(4)]
        mT = sbt("mT", [128, 16, 512], BF16); TmT = T()
        bot = sbt("bot", [128, 8, 512], BF16); Tbot = T()
        h2T16 = sbt("h2T16", [128, 16, 512], BF16); Th16 = T()
        h2T32 = sbt("h2T32", [128, 16, 128], F32); Th32 = T()
        h2 = sbt("h2", [128, D], F32); Th2 = T()
        hidT = sbt("hidT", [128, 4, 512], BF16); Thid = T()
        wbig = Rot(st, nc, "wbig", [128, 8192], BF16, 4)
        wbb = Rot(st, nc, "wbb", [128, 8, 128], BF16, 2)
        gt = Rot(st, nc, "gt", [128, 512], F32, 2)
        mt = Rot(st, nc, "mt", [128, 512], F32, 2)
        sl = Rot(st, nc, "sl", [128, 512], F32, 2)
        junk = Rot(st, nc, "junkc", [128, D], BF16, 1)
        smr = Rot(st, nc, "smr", [128, 64], F32, 2)
        psm = Rot(st, nc, "psmc", [128, 512], F32, 5, psum=True)
        psr = Rot(st, nc, "psr", [128, 32], F32, 1, psum=True)

        S.op("sp", lambda h: h.dma_start(out=ident[:], in_=ident_d), writes=[Tid], dma=True)
        S.op("sp", lambda h: h.dma_start(out=g2bc[:], in_=g2.partition_broadcast(128)), writes=[Tg], dma=True)
        S.op("sp", lambda h: h.dma_start(out=fgbc[:], in_=fg.partition_broadcast(128)), writes=[Tg], dma=True)
        S.op("sp", lambda h: h.dma_start(out=wr[:].rearrange("p c n -> p (c n)"), in_=wrd), writes=[Twr], dma=True)
        S.op("sp", lambda h: h.dma_start(out=brbc[:], in_=brd.partition_broadcast(128)), writes=[Twr], dma=True)
        S.op("dve", lambda h: h.memset(eps[:], RMS_EPS), writes=[Tg])

        def rms(tt, dst_ap, gbc, Tdst_w):
            sm, Tsm = smr.next(); jb, Tj = junk.next()
            S.op("act", lambda h: h.activation(out=jb[:], in_=x1[tt][:], func=AF.Square, accum_out=sm[:, 0:1]), reads=[Tx1[tt]], writes=[Tj, Tsm])
            S.op("act", lambda h: h.activation(out=sm[:, 1:2], in_=sm[:, 0:1], func=AF.Sqrt, bias=eps[:, 0:1], scale=1.0 / D), reads=[Tsm, Tg], writes=[Tsm])
            S.op("dve", lambda h: h.reciprocal(out=sm[:, 2:3], in_=sm[:, 1:2]), reads=[Tsm], writes=[Tsm])
            S.op("dve", lambda h: h.scalar_tensor_tensor(out=dst_ap, in0=x1[tt][:], scalar=sm[:, 2:3], in1=gbc[:], op0=ALU.mult, op1=ALU.mult),
                 reads=[Tx1[tt], Tsm, Tg], writes=Tdst_w)

        def do_group(tg):
            cols = slice(tg * 512, (tg + 1) * 512)
            S.op("sp", lambda h: h.dma_start(out=bot[:], in_=BOT[:, cols].rearrange("(c p) t -> p c t", p=128)), writes=[Tbot], dma=True)
            for tt in range(4):
                S.op("sp", lambda h, tt=tt: h.dma_start(out=x1[tt][:], in_=x[tg * 512 + tt * 128:tg * 512 + (tt + 1) * 128, :]), writes=[Tx1[tt]], dma=True)
            for dc in range(16 if upto >= 1 else 0):
                wb_, Twb = wbb.next()
                S.op("pool", lambda h, wb_=wb_, dc=dc: h.dma_start(out=wb_[:].rearrange("p c n -> p (c n)"), in_=wbd[dc]), writes=[Twb], dma=True)
                pb, Tp = psm.next()
                _mm_group(S, pb[:], [(wb_[:, c, :], bot[:, c, :]) for c in range(8)], [Twb, Tbot], Tp)
                g_, Tg_ = gt.next(); m_, Tm_ = mt.next()
                S.op("sp", lambda h, g_=g_, dc=dc: h.dma_start(out=g_[:], in_=G1T[dc * 128:(dc + 1) * 128, cols]), writes=[Tg_], dma=True)
                S.op("sp", lambda h, m_=m_, dc=dc: h.dma_start(out=m_[:], in_=M0T[dc * 128:(dc + 1) * 128, cols]), writes=[Tm_], dma=True)
                S.op("dve", lambda h, g_=g_, pb=pb: h.tensor_tensor(out=g_[:], in0=pb[:], in1=g_[:], op=ALU.mult), reads=[Tp, Tg_], writes=[Tg_])
                S.op("pool", lambda h, g_=g_, m_=m_, dc=dc: h.tensor_tensor(out=mT[:, dc, :], in0=g_[:], in1=m_[:], op=ALU.add), reads=[Tg_, Tm_], writes=[TmT])
            for cgp in range(4 if upto >= 2 else 0):
                wo_, Two = wbig.next()
                S.op("pool", lambda h, wo_=wo_, cgp=cgp: h.dma_start(out=wo_[:], in_=wod[cgp]), writes=[Two], dma=True)
                for tt in range(4):
                    pb, Tp = psm.next()
                    _mm_group(S, pb[:], [(mT[:, c, tt * 128:(tt + 1) * 128], wo_[:, c * 512:(c + 1) * 512]) for c in range(16)], [Two, TmT], Tp)
                    S.op("dve", lambda h, pb=pb, tt=tt, cgp=cgp: h.tensor_tensor(out=x1[tt][:, cgp * 512:(cgp + 1) * 512], in0=pb[:], in1=x1[tt][:, cgp * 512:(cgp + 1) * 512], op=ALU.add),
                         reads=[Tp, Tx1[tt]], writes=[Tx1[tt]])
            for tt in range(4 if upto >= 3 else 0):
                rms(tt, h2[:], g2bc, [Th2])
                if upto < 4:
                    continue
                for c4 in range(4):
                    pb, Tp = psm.next()
                    for k in range(4):
                        c = c4 * 4 + k
                        S.op("pe", lambda h, pb=pb, c=c, k=k: h.matmul(pb[:, k * 128:(k + 1) * 128], lhsT=h2[:, c * 128:(c + 1) * 128], rhs=ident[:], start=True, stop=True), reads=[Th2, Tid], writes=[Tp])
                    src = pb[:].rearrange("p (k t) -> p k t", k=4)
                    S.op("act", lambda h, src=src, c4=c4: h.copy(out=h2T32[:, c4 * 4:(c4 + 1) * 4, :], in_=src), reads=[Tp], writes=[Th32])
                    S.op("dve", lambda h, src=src, c4=c4, tt=tt: h.tensor_copy(out=h2T16[:, c4 * 4:(c4 + 1) * 4, tt * 128:(tt + 1) * 128], in_=h2T32[:, c4 * 4:(c4 + 1) * 4, :]), reads=[Th32], writes=[Th16])
                if upto < 5:
                    continue
                pr, Tpr = psr.next()
                _mm_group(S, pr[:, 0:20], [(h2T32[:, c, :], wr[:, c, :]) for c in range(16)], [Th32, Twr], Tpr)
                sm, Tsm = smr.next()
                V = lambda fn, rd=(), sm=sm, Tsm=Tsm: S.op("dve", fn, reads=[Tsm] + list(rd), writes=[Tsm])
                S.op("dve", lambda h, sm=sm, pr=pr: h.tensor_tensor(out=sm[:, 0:20], in0=pr[:, 0:20], in1=brbc[:], op=ALU.add), reads=[Tpr, Twr], writes=[Tsm])
                V(lambda h, sm=sm: h.reduce_max(out=sm[:, 20:21], in_=sm[:, 0:4], axis=AX))
                V(lambda h, sm=sm: h.tensor_scalar(out=sm[:, 21:25], in0=sm[:, 0:4], scalar1=sm[:, 20:21], scalar2=None, op0=ALU.is_ge))
                V(lambda h, sm=sm: h.tensor_scalar(out=sm[:, 25:26], in0=sm[:, 20:21], scalar1=-1.0, scalar2=None, op0=ALU.mult))
                S.op("act", lambda h, sm=sm: h.activation(out=sm[:, 26:30], in_=sm[:, 0:4], func=AF.Exp, bias=sm[:, 25:26], scale=1.0, accum_out=sm[:, 30:31]), reads=[Tsm], writes=[Tsm])
                V(lambda h, sm=sm: h.reciprocal(out=sm[:, 31:32], in_=sm[:, 30:31]))
                V(lambda h, sm=sm: h.tensor_scalar(out=sm[:, 32:36], in0=sm[:, 4:8], scalar1=sm[:, 21:22], scalar2=None, op0=ALU.mult))
                for g in range(1, 4):
                    V(lambda h, sm=sm, g=g: h.scalar_tensor_tensor(out=sm[:, 32:36], in0=sm[:, 4 + 4 * g:8 + 4 * g], scalar=sm[:, 21 + g:22 + g], in1=sm[:, 32:36], op0=ALU.mult, op1=ALU.add))
                V(lambda h, sm=sm: h.reduce_max(out=sm[:, 36:37], in_=sm[:, 32:36], axis=AX))
                V(lambda h, sm=sm: h.tensor_scalar(out=sm[:, 37:41], in0=sm[:, 32:36], scalar1=sm[:, 36:37], scalar2=None, op0=ALU.is_ge))
                V(lambda h, sm=sm: h.scalar_tensor_tensor(out=sm[:, 41:45], in0=sm[:, 37:41], scalar=-1e30, in1=sm[:, 32:36], op0=ALU.mult, op1=ALU.add))
                V(lambda h, sm=sm: h.reduce_max(out=sm[:, 45:46], in_=sm[:, 41:45], axis=AX))
                V(lambda h, sm=sm: h.tensor_scalar(out=sm[:, 46:50], in0=sm[:, 41:45], scalar1=sm[:, 45:46], scalar2=None, op0=ALU.is_ge))
                V(lambda h, sm=sm: h.tensor_tensor(out=sm[:, 50:51], in0=sm[:, 45:46], in1=sm[:, 36:37], op=ALU.subtract))
                S.op("act", lambda h, sm=sm: h.activation(out=sm[:, 51:52], in_=sm[:, 50:51], func=AF.Exp), reads=[Tsm], writes=[Tsm])
                V(lambda h, sm=sm: h.tensor_scalar(out=sm[:, 52:53], in0=sm[:, 51:52], scalar1=1.0, scalar2=None, op0=ALU.add))
                V(lambda h, sm=sm: h.reciprocal(out=sm[:, 53:54], in_=sm[:, 52:53]))
                V(lambda h, sm=sm: h.tensor_tensor(out=sm[:, 54:55], in0=sm[:, 53:54], in1=sm[:, 31:32], op=ALU.mult))
                V(lambda h, sm=sm: h.tensor_tensor(out=sm[:, 55:56], in0=sm[:, 31:32], in1=sm[:, 54:55], op=ALU.subtract))
                V(lambda h, sm=sm: h.tensor_scalar(out=sm[:, 56:60], in0=sm[:, 37:41], scalar1=sm[:, 54:55], scalar2=None, op0=ALU.mult))
                V(lambda h, sm=sm: h.scalar_tensor_tensor(out=sm[:, 56:60], in0=sm[:, 46:50], scalar=sm[:, 55:56], in1=sm[:, 56:60], op0=ALU.mult, op1=ALU.add))
                for g in range(4):
                    S.op("dve", lambda h, sm=sm, g=g, tt=tt: h.tensor_scalar(out=cw[tt][:, 4 * g:4 * g + 4], in0=sm[:, 56:60], scalar1=sm[:, 21 + g:22 + g], scalar2=None, op0=ALU.mult),
                         reads=[Tsm], writes=[Tcw[tt]])
            for e in range(nexp):
                w1_, Tw1 = wbig.next()
                S.op("pool", lambda h, w1_=w1_, e=e: h.dma_start(out=w1_[:], in_=w1d[e]), writes=[Tw1], dma=True)
                w3_, Tw3 = wbig.next()
                S.op("pool", lambda h, w3_=w3_, e=e: h.dma_start(out=w3_[:], in_=w3d[e]), writes=[Tw3], dma=True)
                w2_, Tw2 = wbig.next()
                S.op("pool", lambda h, w2_=w2_, e=e: h.dma_start(out=w2_[:], in_=w2d[e]), writes=[Tw2], dma=True)
                for fc in range(4):
                    p1, Tp1 = psm.next()
                    _mm_group(S, p1[:], [(w1_[:, c * 512 + fc * 128:c * 512 + (fc + 1) * 128], h2T16[:, c, :]) for c in range(16)], [Tw1, Th16], Tp1)
                    p3, Tp3 = psm.next()
                    _mm_group(S, p3[:], [(w3_[:, c * 512 + fc * 128:c * 512 + (fc + 1) * 128], h2T16[:, c, :]) for c in range(16)], [Tw3, Th16], Tp3)
                    s_, Ts_ = sl.next()
                    S.op("act", lambda h, s_=s_, p1=p1: h.activation(out=s_[:], in_=p1[:], func=AF.Silu), reads=[Tp1], writes=[Ts_])
                    S.op("dve", lambda h, s_=s_, p3=p3, fc=fc: h.tensor_tensor(out=hidT[:, fc, :], in0=p3[:], in1=s_[:], op=ALU.mult), reads=[Tp3, Ts_], writes=[Thid])
                for tt in range(4):
                    for cgp in range(4):
                        py, Tpy = psm.next()
                        _mm_group(S, py[:], [(hidT[:, fc, tt * 128:(tt + 1) * 128], w2_[:, fc * 2048 + cgp * 512:fc * 2048 + (cgp + 1) * 512]) for fc in range(4)], [Tw2, Thid], Tpy)
                        S.op("dve", lambda h, py=py, tt=tt, cgp=cgp, e=e: h.scalar_tensor_tensor(out=x1[tt][:, cgp * 512:(cgp + 1) * 512], in0=py[:], scalar=cw[tt][:, e:e + 1], in1=x1[tt][:, cgp * 512:(cgp + 1) * 512], op0=ALU.mult, op1=ALU.add),
                             reads=[Tpy, Tcw[tt], Tx1[tt]], writes=[Tx1[tt]])
            for tt in range(4):
                rows = slice(tg * 512 + tt * 128, tg * 512 + (tt + 1) * 128)
                if final:
                    rms(tt, h2[:], fgbc, [Th2])
                    S.op("sp", lambda h, rows=rows: h.dma_start(out=XO[rows, :], in_=h2[:]), reads=[Th2], dma=True)
                else:
                    S.op("sp", lambda h, rows=rows, tt=tt: h.dma_start(out=XO[rows, :], in_=x1[tt][:]), reads=[Tx1[tt]], dma=True)

        for tg in range(ngroups):
            do_group(tg)
        S.emit(st)
    return nc


def prep_C(l, inp):
    wrr = np.concatenate([np.asarray(inp["router_g_w"][l]), np.asarray(inp["router_e_w"][l])], axis=1)
    return {
        "wb": _tile_km(np.asarray(inp["w_proj_b"][l]), 128),
        "wo": _tile_km(np.asarray(inp["w_out"][l]), 512),
        "g2": np.asarray(inp["norm2_g"][l]).reshape(1, D),
        "fg": np.asarray(inp["final_g"]).reshape(1, D),
        "wr": np.ascontiguousarray(wrr.reshape(16, 128, 20).transpose(1, 0, 2)).reshape(128, 320),
        "br": np.concatenate([np.asarray(inp["router_g_b"][l]), np.asarray(inp["router_e_b"][l])]).reshape(1, 20),
        "w1t": np.stack([_tile_km(np.asarray(inp["w1"][l][e]), 512)[0] for e in range(NEXP)]),
        "w3t": np.stack([_tile_km(np.asarray(inp["w3"][l][e]), 512)[0] for e in range(NEXP)]),
        "w2t": np.stack([_tile_km(np.asarray(inp["w2"][l][e]), 2048)[0] for e in range(NEXP)]),
        "ident": np.eye(128, dtype=np.float32),
    }


_PROG = {}


def _prog(key, fn):
    if key not in _PROG:
        _PROG[key] = fn()
    return _PROG[key]


def kernel(**inp):
    cores = list(range(NCORE))
    x = np.ascontiguousarray(np.asarray(inp["x"], dtype=np.float32).reshape(NCORE, NT, D))
    xs = [x[c] for c in cores]
    depth = inp["w_in"].shape[0]
    for l in range(depth):
        lam_init = 0.8 - 0.6 * math.exp(-0.3 * l)
        pa = prep_A(l, inp)
        ra = run_bass_kernel_spmd(_prog("A", build_A), [dict(pa, x=xs[c]) for c in cores], core_ids=cores).results
        del pa
        rb = run_bass_kernel_spmd(_prog(("B", l), lambda: build_B(lam_init)), [prep_B(l, inp, ra, c) for c in cores], core_ids=cores).results
        pc = prep_C(l, inp)
        final = (l == depth - 1)
        rc = run_bass_kernel_spmd(_prog(("C", final), lambda: build_C(final)),
                                  [dict(pc, x=xs[c], M0T=ra[c]["M0T"], G1T=ra[c]["G1T"], BOT=rb[c]["BOT"]) for c in cores], core_ids=cores).results
        xs = [np.asarray(rc[c]["XO"]) for c in cores]
        del pc, ra, rb, rc
    return np.stack(xs).reshape(inp["x"].shape).astype(np.float32)
```

```python
import math
from contextlib import ExitStack

import numpy as np
import ml_dtypes
import concourse.bass as bass
import concourse.mybir as mybir
from concourse.bass_utils import run_bass_kernel_spmd

F32 = mybir.dt.float32
BF16 = mybir.dt.bfloat16
AF = mybir.ActivationFunctionType
ALU = mybir.AluOpType
NPBF = ml_dtypes.bfloat16

D = 2048
NCORE = 8
NT = 4096
SEQ = 16384
NH = 8
RMS_EPS = 1e-6
LN_EPS = 1e-5
NEXP = 16
FF = 512

EPOCH = 16000
NDMASEM = 8
SAME_ENGINE_SYNC = True


class T:
    __slots__ = ("w", "r")

    def __init__(self):
        self.w = None
        self.r = []


class Op:
    __slots__ = ("eng", "fn", "dma", "deps", "signal", "tick", "idx", "dsem", "dval", "prev_dma")

    def __init__(self, eng, fn, dma):
        self.eng = eng
        self.fn = fn
        self.dma = dma
        self.deps = []
        self.signal = False
        self.tick = 0
        self.idx = 0
        self.dsem = None
        self.dval = 0
        self.prev_dma = None


class Sched:
    ENGS = ("pe", "act", "dve", "pool", "sp")

    def __init__(self, nc):
        self.nc = nc
        self.ops = {e: [] for e in self.ENGS}
        self.ndma = {e: 0 for e in self.ENGS}
        self.dma_hist = {e: [] for e in self.ENGS}

    def op(self, eng, fn, reads=(), writes=(), dma=False):
        o = Op(eng, fn, dma)
        o.idx = len(self.ops[eng])
        deps = {}

        def add(d):
            if d is None or d is o:
                return
            if (not d.dma) and d.eng == eng:
                if not o.dma and (eng == "pe" or not SAME_ENGINE_SYNC):
                    return
            if d.dma:
                deps[("dma", id(d))] = d
            else:
                k = ("c", d.eng)
                if k not in deps or deps[k].idx < d.idx:
                    deps[k] = d

        for t in reads:
            add(t.w)
        for t in writes:
            add(t.w)
            for rr in t.r:
                add(rr)
        o.deps = list(deps.values())
        for d in o.deps:
            d.signal = True
        for t in reads:
            t.r.append(o)
            if len(t.r) > 48:
                keep = {}
                for rr in t.r:
                    if rr.dma:
                        keep[id(rr)] = rr
                    elif rr.eng not in keep or keep[rr.eng].idx < rr.idx:
                        keep[rr.eng] = rr
                t.r = list(keep.values())
        for t in writes:
            t.w = o
            t.r = []
        if dma:
            o.signal = True
            j = self.ndma[eng]
            self.ndma[eng] += 1
            hist = self.dma_hist[eng]
            if j >= NDMASEM:
                o.prev_dma = hist[j - NDMASEM]
            hist.append(o)
            o.dval = 16 * (j // NDMASEM + 1)
            o.dsem = j % NDMASEM
        self.ops[eng].append(o)
        return o

    def emit(self, stack):
        nc = self.nc
        nepoch = {}
        for e in self.ENGS:
            c = 0
            for o in self.ops[e]:
                if o.signal and not o.dma:
                    c += 1
                    o.tick = c
            nepoch[e] = (c + EPOCH - 1) // EPOCH
        csem = {e: [stack.enter_context(nc.semaphore(f"c_{e}_{k}")) for k in range(nepoch[e])]
                for e in self.ENGS}
        dsem = {e: ([stack.enter_context(nc.semaphore(f"d_{e}_{k}")) for k in range(NDMASEM)]
                    if self.ndma[e] else []) for e in self.ENGS}

        def semval(d):
            if d.dma:
                return dsem[d.eng][d.dsem], d.dval
            k = (d.tick - 1) // EPOCH
            return csem[d.eng][k], d.tick - k * EPOCH

        block = stack.enter_context(nc.Block())
        reg = {"pe": block.tensor, "act": block.scalar, "dve": block.vector,
               "pool": block.gpsimd, "sp": block.sync}

        def make(e):
            def body(h):
                waited = {}
                for o in self.ops[e]:
                    ws = [semval(d) for d in o.deps]
                    if o.prev_dma is not None:
                        ws.append(semval(o.prev_dma))
                    for s, v in ws:
                        if waited.get(id(s), 0) >= v:
                            continue
                        waited[id(s)] = v
                        h.wait_ge(s, v)
                    ins = o.fn(h)
                    if o.signal:
                        s, v = semval(o)
                        ins.then_inc(s, 16 if o.dma else 1)
                for o in self.dma_hist[e][-NDMASEM:]:
                    s, v = semval(o)
                    if waited.get(id(s), 0) < v:
                        waited[id(s)] = v
                        h.wait_ge(s, v)
            return body

        for e in self.ENGS:
            if self.ops[e]:
                reg[e](make(e))


class Rot:
    def __init__(self, st, nc, name, shape, dt, n, psum=False):
        self.bufs = []
        for i in range(n):
            if psum:
                b = st.enter_context(nc.psum_tensor(f"{name}{i}", shape, dt))
            else:
                b = st.enter_context(nc.sbuf_tensor(f"{name}{i}", shape, dt))
            self.bufs.append((b, T()))
        self.i = 0

    def next(self):
        r = self.bufs[self.i % len(self.bufs)]
        self.i += 1
        return r


def _mm_group(S, out_ap, pairs, reads, tout):
    n = len(pairs)
    for k, (l, r) in enumerate(pairs):
        S.op("pe", (lambda h, l=l, r=r, k=k: h.matmul(out_ap, lhsT=l, rhs=r, start=(k == 0), stop=(k == n - 1))),
             reads=reads, writes=[tout])


def build_A():
    nc = bass.Bass("TRN2", target_bir_lowering=False)
    dr = lambda n, s, d, k: nc.dram_tensor(n, s, d, kind=k).ap()
    x = dr("x", [NT, D], F32, "ExternalInput")
    g1 = dr("g1", [1, D], F32, "ExternalInput")
    wf = dr("wf", [72, 128, 16 * 128], F32, "ExternalInput")
    wt = dr("wt", [2, 128, 16 * 1024], F32, "ExternalInput")
    bg = dr("bg", [128, 32], F32, "ExternalInput")
    lng = dr("lng", [1, 1024], F32, "ExternalInput")
    lnb = dr("lnb", [1, 1024], F32, "ExternalInput")
    wst = dr("wst", [128, 8 * 128], F32, "ExternalInput")
    sb_ = dr("sgub", [1, 1024], F32, "ExternalInput")
    wa = dr("wa", [16, 128, 8 * 128], F32, "ExternalInput")
    ident_d = dr("ident", [128, 128], F32, "ExternalInput")
    QT = dr("QT", [1024, NT], BF16, "ExternalOutput")
    KT = dr("KT", [1024, NT], BF16, "ExternalOutput")
    VV = dr("VV", [NT, 1024], BF16, "ExternalOutput")
    M0T = dr("M0T", [D, NT], F32, "ExternalOutput")
    G1T = dr("G1T", [D, NT], F32, "ExternalOutput")
    S = Sched(nc)
    HALF = 1024
    with ExitStack() as st:
        sbt = lambda n, s, d: st.enter_context(nc.sbuf_tensor(n, s, d))
        ident = sbt("identb", [128, 128], BF16); Tid = T()
        g1bc = sbt("g1bc", [128, D], F32); Tg1 = T()
        lngbc = sbt("lngbc", [128, 1024], F32); lnbbc = sbt("lnbbc", [128, 1024], F32); Tln = T()
        bsb = sbt("bsb", [128, 1024], F32); Tbsb = T()
        wsT = sbt("wsT", [128, 1024], BF16); Tws = T()
        bgt = sbt("bgt", [128, 32], F32); Tbg = T()
        epsr = sbt("epsr", [128, 1], F32); epsl = sbt("epsl", [128, 1], F32); Teps = T()
        hT = sbt("hT", [128, 16, HALF], BF16); ThT = [T() for _ in range(2)]
        guT = sbt("guT", [128, 8, HALF], BF16); Tgu = [T() for _ in range(2)]
        aT = sbt("aT", [128, 8, HALF], BF16); TaT = [T() for _ in range(2)]
        wtb = sbt("wtb", [128, 16, 1024], BF16); Twt = T()
        xr = Rot(st, nc, "xin", [128, D], F32, 2)
        h16 = Rot(st, nc, "h16", [128, D], BF16, 2)
        junk = Rot(st, nc, "junk", [128, D], BF16, 1)
        sm = Rot(st, nc, "sm", [128, 8], F32, 4)
        wfb = Rot(st, nc, "wfb", [128, 16, 128], BF16, 3)
        wab = Rot(st, nc, "wab", [128, 8, 128], BF16, 2)
        stg16 = Rot(st, nc, "stg16", [128, 1024], BF16, 3)
        stg32 = Rot(st, nc, "stg32", [128, 512], F32, 4)
        gv = Rot(st, nc, "gv", [128, 1024], F32, 2)
        vln = Rot(st, nc, "vln", [128, 1024], BF16, 2)
        tmp32 = Rot(st, nc, "tmp32", [128, 1024], F32, 2)
        pst = Rot(st, nc, "pst", [128, 512], BF16, 2, psum=True)
        psm = Rot(st, nc, "psm", [128, 512], F32, 4, psum=True)
        psw = Rot(st, nc, "psw", [128, 1024], F32, 1, psum=True)

        S.op("sp", lambda h: h.dma_start(out=g1bc[:], in_=g1.partition_broadcast(128)), writes=[Tg1], dma=True)
        S.op("sp", lambda h: h.dma_start(out=lngbc[:], in_=lng.partition_broadcast(128)), writes=[Tln], dma=True)
        S.op("sp", lambda h: h.dma_start(out=lnbbc[:], in_=lnb.partition_broadcast(128)), writes=[Tln], dma=True)
        S.op("sp", lambda h: h.dma_start(out=bsb[:], in_=sb_.partition_broadcast(128)), writes=[Tbsb], dma=True)
        S.op("sp", lambda h: h.dma_start(out=bgt[:], in_=bg), writes=[Tbg], dma=True)
        S.op("pool", lambda h: h.dma_start(out=ident[:], in_=ident_d), writes=[Tid], dma=True)
        S.op("pool", lambda h: h.dma_start(out=wsT[:], in_=wst), writes=[Tws], dma=True)
        S.op("dve", lambda h: h.memset(epsr[:], RMS_EPS), writes=[Teps])
        S.op("dve", lambda h: h.memset(epsl[:], LN_EPS), writes=[Teps])

        def do_half(half):
            t0 = half * HALF
            for tt in range(8):
                tg = tt // 4
                xb, Tx = xr.next()
                rows = slice(t0 + tt * 128, t0 + (tt + 1) * 128)
                S.op("sp", lambda h, xb=xb, rows=rows: h.dma_start(out=xb[:], in_=x[rows, :]), writes=[Tx], dma=True)
                jb, Tj = junk.next(); smb, Tsm = sm.next()
                S.op("act", lambda h, xb=xb, jb=jb, smb=smb: h.activation(out=jb[:], in_=xb[:], func=AF.Square, accum_out=smb[:, 0:1]),
                     reads=[Tx], writes=[Tj, Tsm])
                S.op("act", lambda h, smb=smb: h.activation(out=smb[:, 1:2], in_=smb[:, 0:1], func=AF.Sqrt, bias=epsr[:, 0:1], scale=1.0 / D),
                     reads=[Tsm, Teps], writes=[Tsm])
                S.op("dve", lambda h, smb=smb: h.reciprocal(out=smb[:, 2:3], in_=smb[:, 1:2]), reads=[Tsm], writes=[Tsm])
                hb, Th = h16.next()
                S.op("dve", lambda h, hb=hb, xb=xb, smb=smb: h.scalar_tensor_tensor(out=hb[:], in0=xb[:], scalar=smb[:, 2:3], in1=g1bc[:], op0=ALU.mult, op1=ALU.mult),
                     reads=[Tx, Tsm, Tg1], writes=[Th])
                for c4 in range(4):
                    pb, Tp = pst.next()
                    for k in range(4):
                        c = c4 * 4 + k
                        S.op("pe", lambda h, pb=pb, hb=hb, c=c, k=k: h.transpose(pb[:, k * 128:(k + 1) * 128], hb[:, c * 128:(c + 1) * 128], ident[:]),
                             reads=[Th, Tid], writes=[Tp])
                    eng = "act" if c4 % 2 == 0 else "dve"
                    dst = hT[:, c4 * 4:(c4 + 1) * 4, tt * 128:(tt + 1) * 128]
                    src = pb[:].rearrange("p (k t) -> p k t", k=4)
                    if eng == "act":
                        S.op("act", lambda h, dst=dst, src=src: h.copy(out=dst, in_=src), reads=[Tp], writes=[ThT[tg]])
                    else:
                        S.op("dve", lambda h, dst=dst, src=src: h.tensor_copy(out=dst, in_=src), reads=[Tp], writes=[ThT[tg]])

            def fm_group(cg, evac):
                wb, Tw = wfb.next()
                S.op("pool", lambda h, wb=wb, cg=cg: h.dma_start(out=wb[:].rearrange("p c n -> p (c n)"), in_=wf[cg]), writes=[Tw], dma=True)
                for tg in range(2):
                    pb, Tp = psm.next()
                    _mm_group(S, pb[:], [(wb[:, c, :], hT[:, c, tg * 512:(tg + 1) * 512]) for c in range(16)], [Tw, ThT[tg]], Tp)
                    evac(tg, pb, Tp)

            for cg in range(8):
                def ev(tg, pb, Tp, cg=cg):
                    S.op("act", lambda h: h.activation(out=guT[:, cg, tg * 512:(tg + 1) * 512], in_=pb[:], func=AF.Gelu_apprx_tanh),
                         reads=[Tp], writes=[Tgu[tg]])
                fm_group(cg, ev)
            for q in range(4):
                S.op("pool", lambda h, q=q: h.dma_start(out=wtb[:, q * 4:(q + 1) * 4, :].rearrange("p c n -> p (c n)"), in_=wt[0, :, q * 4096:(q + 1) * 4096]),
                     writes=[Twt], dma=True)
            for tt in range(8):
                tg = tt // 4
                ts = slice(tt * 128, (tt + 1) * 128)
                gb, Tgv = gv.next(); smb, Tsm = sm.next()
                pw, Tpw = psw.next()
                for hh in range(2):
                    _mm_group(S, pw[:, hh * 512:(hh + 1) * 512], [(hT[:, c, ts], wtb[:, c, hh * 512:(hh + 1) * 512]) for c in range(16)], [Twt, ThT[tg]], Tpw)
                S.op("act", lambda h, gb=gb, pw=pw, smb=smb: h.activation(out=gb[:], in_=pw[:], func=AF.Gelu_apprx_tanh, accum_out=smb[:, 0:1]),
                     reads=[Tpw], writes=[Tgv, Tsm])
                jb, Tj = junk.next()
                S.op("act", lambda h, gb=gb, jb=jb, smb=smb: h.activation(out=jb[:, 0:1024], in_=gb[:], func=AF.Square, accum_out=smb[:, 1:2]),
                     reads=[Tgv], writes=[Tj, Tsm])
                S.op("dve", lambda h, smb=smb: h.tensor_scalar(out=smb[:, 2:3], in0=smb[:, 0:1], scalar1=1.0 / 1024, scalar2=None, op0=ALU.mult), reads=[Tsm], writes=[Tsm])
                S.op("dve", lambda h, smb=smb: h.tensor_tensor(out=smb[:, 3:4], in0=smb[:, 2:3], in1=smb[:, 2:3], op=ALU.mult), reads=[Tsm], writes=[Tsm])
                S.op("dve", lambda h, smb=smb: h.scalar_tensor_tensor(out=smb[:, 4:5], in0=smb[:, 1:2], scalar=1.0 / 1024, in1=smb[:, 3:4], op0=ALU.mult, op1=ALU.subtract),
                     reads=[Tsm], writes=[Tsm])
                S.op("act", lambda h, smb=smb: h.activation(out=smb[:, 5:6], in_=smb[:, 4:5], func=AF.Sqrt, bias=epsl[:, 0:1], scale=1.0), reads=[Tsm, Teps], writes=[Tsm])
                S.op("dve", lambda h, smb=smb: h.reciprocal(out=smb[:, 6:7], in_=smb[:, 5:6]), reads=[Tsm], writes=[Tsm])
                tb, Tt = tmp32.next()
                S.op("dve", lambda h, tb=tb, gb=gb, smb=smb: h.tensor_scalar(out=tb[:], in0=gb[:], scalar1=smb[:, 2:3], scalar2=smb[:, 6:7], op0=ALU.subtract, op1=ALU.mult),
                     reads=[Tgv, Tsm], writes=[Tt])
                S.op("pool", lambda h, tb=tb: h.tensor_tensor(out=tb[:], in0=tb[:], in1=lngbc[:], op=ALU.mult), reads=[Tt, Tln], writes=[Tt])
                vb, Tv = vln.next()
                S.op("dve", lambda h, tb=tb, vb=vb: h.tensor_tensor(out=vb[:], in0=tb[:], in1=lnbbc[:], op=ALU.add), reads=[Tt, Tln], writes=[Tv])
                pw2, Tpw2 = psw.next()
                for g in range(8):
                    S.op("pe", lambda h, pw2=pw2, vb=vb, g=g: h.matmul(pw2[:, g * 128:(g + 1) * 128], lhsT=vb[:, g * 128:(g + 1) * 128], rhs=wsT[:, g * 128:(g + 1) * 128], start=True, stop=True),
                         reads=[Tv, Tws], writes=[Tpw2])
                tb2, Tt2 = tmp32.next()
                S.op("dve", lambda h, tb2=tb2, pw2=pw2: h.tensor_tensor(out=tb2[:], in0=pw2[:], in1=bsb[:], op=ALU.add), reads=[Tpw2, Tbsb], writes=[Tt2])
                S.op("pool", lambda h, tb2=tb2, ts=ts: h.tensor_tensor(out=aT[:, :, ts], in0=tb2[:].rearrange("p (g t) -> p g t", g=8), in1=guT[:, :, ts], op=ALU.mult),
                     reads=[Tt2, Tgu[tg]], writes=[TaT[tg]])
            for which, base_cg, dst, scl in (("q", 16, QT, 0.125), ("k", 24, KT, 1.0)):
                for hh in range(8):
                    def ev(tg, pb, Tp, hh=hh, dst=dst, scl=scl):
                        sb2, Ts = stg16.next()
                        S.op("act", lambda h: h.activation(out=sb2[:, 0:512], in_=pb[:], func=AF.Copy, scale=scl), reads=[Tp], writes=[Ts])
                        S.op("sp", lambda h: h.dma_start(out=dst[hh * 128:(hh + 1) * 128, t0 + tg * 512:t0 + (tg + 1) * 512], in_=sb2[:, 0:512]), reads=[Ts], dma=True)
                    fm_group(base_cg + hh, ev)
            for q in range(4):
                S.op("pool", lambda h, q=q: h.dma_start(out=wtb[:, q * 4:(q + 1) * 4, :].rearrange("p c n -> p (c n)"), in_=wt[1, :, q * 4096:(q + 1) * 4096]),
                     writes=[Twt], dma=True)
            for tt in range(8):
                tg = tt // 4
                ts = slice(tt * 128, (tt + 1) * 128)
                pw, Tpw = psw.next()
                for hh in range(2):
                    _mm_group(S, pw[:, hh * 512:(hh + 1) * 512], [(hT[:, c, ts], wtb[:, c, hh * 512:(hh + 1) * 512]) for c in range(16)], [Twt, ThT[tg]], Tpw)
                sb2, Ts = stg16.next()
                S.op("dve", lambda h, sb2=sb2, pw=pw: h.tensor_copy(out=sb2[:], in_=pw[:]), reads=[Tpw], writes=[Ts])
                S.op("sp", lambda h, sb2=sb2, tt=tt: h.dma_start(out=VV[t0 + tt * 128:t0 + (tt + 1) * 128, :], in_=sb2[:]), reads=[Ts], dma=True)
            for dc in range(16):
                def ev(tg, pb, Tp, dc=dc):
                    sb3, Ts = stg32.next()
                    S.op("act", lambda h: h.activation(out=sb3[:], in_=pb[:], func=AF.Sigmoid, bias=bgt[:, 16 + dc:17 + dc], scale=1.0), reads=[Tp, Tbg], writes=[Ts])
                    S.op("sp", lambda h: h.dma_start(out=G1T[dc * 128:(dc + 1) * 128, t0 + tg * 512:t0 + (tg + 1) * 512], in_=sb3[:]), reads=[Ts], dma=True)
                fm_group(56 + dc, ev)
            for dc in range(16):
                wab_, Twa = wab.next()
                S.op("pool", lambda h, wab_=wab_, dc=dc: h.dma_start(out=wab_[:].rearrange("p c n -> p (c n)"), in_=wa[dc]), writes=[Twa], dma=True)
                def ev(tg, pb, Tp, dc=dc, wab_=wab_, Twa=Twa):
                    sb3, Ts = stg32.next()
                    S.op("act", lambda h: h.activation(out=sb3[:], in_=pb[:], func=AF.Sigmoid, bias=bgt[:, dc:dc + 1], scale=1.0), reads=[Tp, Tbg], writes=[Ts])
                    pb2, Tp2 = psm.next()
                    _mm_group(S, pb2[:], [(wab_[:, c, :], aT[:, c, tg * 512:(tg + 1) * 512]) for c in range(8)], [Twa, TaT[tg]], Tp2)
                    S.op("dve", lambda h: h.tensor_tensor(out=sb3[:], in0=pb2[:], in1=sb3[:], op=ALU.mult), reads=[Tp2, Ts], writes=[Ts])
                    S.op("sp", lambda h: h.dma_start(out=M0T[dc * 128:(dc + 1) * 128, t0 + tg * 512:t0 + (tg + 1) * 512], in_=sb3[:]), reads=[Ts], dma=True)
                fm_group(40 + dc, ev)
        for half in range(4):
            do_half(half)
        S.emit(st)
    return nc


def _tile_km(w, ncols):
    K, N = w.shape
    return np.ascontiguousarray(w.reshape(K // 128, 128, N // ncols, ncols).transpose(2, 1, 0, 3)).reshape(N // ncols, 128, (K // 128) * ncols)


def prep_A(l, inp):
    w_in = np.asarray(inp["w_in"][l])
    wf = _tile_km(w_in, 128)
    wt = np.stack([_tile_km(w_in[:, 1024:2048], 1024)[0], _tile_km(w_in[:, 4096:5120], 1024)[0]])
    bg = np.ascontiguousarray(np.asarray(inp["b_gate"][l]).reshape(32, 128).T)
    wst = np.ascontiguousarray(np.asarray(inp["sgu_w"][l]).transpose(2, 0, 1)).reshape(128, 1024)
    return {
        "g1": np.asarray(inp["norm1_g"][l]).reshape(1, D),
        "wf": wf, "wt": wt, "bg": bg,
        "lng": np.asarray(inp["sgu_ln_g"][l]).reshape(1, 1024),
        "lnb": np.asarray(inp["sgu_ln_b"][l]).reshape(1, 1024),
        "wst": wst,
        "sgub": np.asarray(inp["sgu_b"][l]).reshape(1, 1024),
        "wa": _tile_km(np.asarray(inp["w_proj_a"][l]), 128),
        "ident": np.eye(128, dtype=np.float32),
    }


def build_B(lam_init, heads=NH, nqs=8):
    nc = bass.Bass("TRN2", target_bir_lowering=False)
    dr = lambda n, s, d, k: nc.dram_tensor(n, s, d, kind=k).ap()
    QA = dr("QA", [NH, 2, 68, NT], BF16, "ExternalInput")
    KA = dr("KA", [NH, 2, 64, SEQ], BF16, "ExternalInput")
    sigt = dr("sigt", [8, 2, SEQ], BF16, "ExternalInput")
    VH = dr("VH", [NH, 128, 128 * 129], BF16, "ExternalInput")
    btab_d = dr("btab", [128, NH * 8 * 128], F32, "ExternalInput")
    dmat_d = dr("dmat", [128, 4 * 1024], F32, "ExternalInput")
    negi_d = dr("negi", [128, 1024], F32, "ExternalInput")
    lamv = dr("lamv", [4, 64], F32, "ExternalInput")
    gdn = dr("gdn", [1, 128], F32, "ExternalInput")
    ident_d = dr("ident", [128, 128], F32, "ExternalInput")
    BOT = dr("BOT", [1024, NT], BF16, "ExternalOutput")
    slopes = [2.0 ** (-(i + 1)) for i in range(NH)]
    S = Sched(nc)
    with ExitStack() as st:
        sbt = lambda n, s, d: st.enter_context(nc.sbuf_tensor(n, s, d))
        ident = sbt("identb", [128, 128], BF16); Tid = T()
        btab = sbt("btabs", [128, NH * 8 * 128], F32); Tbt = T()
        dmat = sbt("dmats", [128, 4 * 1024], F32); Tdm = T()
        negi = sbt("negis", [128, 1024], F32); Tng = T()
        lv = sbt("lv", [128, 4 * 64], F32); lsm = sbt("lsm", [128, 8], F32); Tlam = T()
        gbc = sbt("gbc", [128, 128], F32); Tg = T()
        eps = sbt("epsb", [128, 1], F32)
        K1 = sbt("K1", [68, SEQ], BF16); K2 = sbt("K2", [68, SEQ], BF16); TK = [T(), T()]; Tsig = [T(), T()]
        Vs = sbt("Vs", [128, 128 * 129], BF16); TV = [T(), T()]
        Q1 = sbt("Q1", [68, NT], BF16); Q2 = sbt("Q2", [68, NT], BF16); TQ = T()
        PTr = Rot(st, nc, "PT", [128, 1024], BF16, 3)
        psS = Rot(st, nc, "psS", [128, 1024], F32, 2, psum=True)
        accp = st.enter_context(nc.psum_tensor("accp", [128, 3 * 512], F32)); Tacc = T()
        pst = st.enter_context(nc.psum_tensor("pstB", [128, 512], BF16)); Tpst = T()
        esm = Rot(st, nc, "esm", [128, 8], F32, 4)
        t2r = Rot(st, nc, "t2r", [128, 128], F32, 2)
        o32 = Rot(st, nc, "o32", [128, 128], F32, 2)
        ob16 = Rot(st, nc, "ob16", [128, 128], BF16, 2)
        junk = Rot(st, nc, "junkb", [128, 128], F32, 1)
        stg = Rot(st, nc, "stgb", [128, 512], BF16, 2)

        S.op("sp", lambda h: h.dma_start(out=btab[:], in_=btab_d), writes=[Tbt], dma=True)
        S.op("sp", lambda h: h.dma_start(out=dmat[:], in_=dmat_d), writes=[Tdm], dma=True)
        S.op("sp", lambda h: h.dma_start(out=negi[:], in_=negi_d), writes=[Tng], dma=True)
        S.op("pool", lambda h: h.dma_start(out=ident[:], in_=ident_d), writes=[Tid], dma=True)
        for i in range(4):
            S.op("sp", lambda h, i=i: h.dma_start(out=lv[:, i * 64:(i + 1) * 64], in_=lamv[i:i + 1, :].partition_broadcast(128)), writes=[Tlam], dma=True)
        S.op("sp", lambda h: h.dma_start(out=gbc[:], in_=gdn.partition_broadcast(128)), writes=[Tg], dma=True)
        S.op("dve", lambda h: h.memset(eps[:], RMS_EPS), writes=[Tg])
        S.op("dve", lambda h: h.tensor_scalar(out=gbc[:], in0=gbc[:], scalar1=1.0 - lam_init, scalar2=None, op0=ALU.mult), reads=[Tg], writes=[Tg])
        S.op("dve", lambda h: h.tensor_tensor(out=lv[:, 0:64], in0=lv[:, 0:64], in1=lv[:, 64:128], op=ALU.mult), reads=[Tlam], writes=[Tlam])
        S.op("dve", lambda h: h.tensor_tensor(out=lv[:, 128:192], in0=lv[:, 128:192], in1=lv[:, 192:256], op=ALU.mult), reads=[Tlam], writes=[Tlam])
        S.op("dve", lambda h: h.reduce_sum(out=lsm[:, 0:1], in_=lv[:, 0:64], axis=mybir.AxisListType.X), reads=[Tlam], writes=[Tlam])
        S.op("dve", lambda h: h.reduce_sum(out=lsm[:, 1:2], in_=lv[:, 128:192], axis=mybir.AxisListType.X), reads=[Tlam], writes=[Tlam])
        S.op("act", lambda h: h.activation(out=lsm[:, 2:4], in_=lsm[:, 0:2], func=AF.Exp), reads=[Tlam], writes=[Tlam])
        S.op("dve", lambda h: h.tensor_tensor(out=lsm[:, 4:5], in0=lsm[:, 2:3], in1=lsm[:, 3:4], op=ALU.subtract), reads=[Tlam], writes=[Tlam])
        S.op("dve", lambda h: h.tensor_scalar(out=lsm[:, 5:6], in0=lsm[:, 4:5], scalar1=lam_init, scalar2=None, op0=ALU.add), reads=[Tlam], writes=[Tlam])
        lam_ap = lsm[:, 5:6]

        def acc_ap(a, lo, hi):
            return accp[:, (a // 3) * 512 + (a % 3) * 129 + lo:(a // 3) * 512 + (a % 3) * 129 + hi]

        def head(hd):
            m = slopes[hd]
            S.op("sp", lambda h: h.dma_start(out=Q1[:], in_=QA[hd, 0]), writes=[TQ], dma=True)
            S.op("sp", lambda h: h.dma_start(out=Q2[:], in_=QA[hd, 1]), writes=[TQ], dma=True)
            for hf in range(2):
                ks = slice(hf * 8192, (hf + 1) * 8192)
                S.op("sp", lambda h, ks=ks: h.dma_start(out=K1[0:64, ks], in_=KA[hd, 0, :, ks]), writes=[TK[hf]], dma=True)
                S.op("sp", lambda h, ks=ks: h.dma_start(out=K2[0:64, ks], in_=KA[hd, 1, :, ks]), writes=[TK[hf]], dma=True)
                vs = slice(hf * 64 * 129, (hf + 1) * 64 * 129)
                S.op("sp", lambda h, vs=vs: h.dma_start(out=Vs[:, vs], in_=VH[hd, :, vs]), writes=[TV[hf]], dma=True)
            def load_sig(qs):
                par = qs % 2
                rows = slice(64 + 2 * par, 66 + 2 * par)
                S.op("sp", lambda h: h.dma_start(out=K1[rows, :], in_=sigt[qs]), writes=[Tsig[par]], dma=True)
                S.op("sp", lambda h: h.dma_start(out=K2[rows, :], in_=sigt[qs]), writes=[Tsig[par]], dma=True)
            load_sig(0)
            load_sig(1)
            def do_qs(qs):
                qsl = slice(qs * 512, (qs + 1) * 512)
                par = qs % 2
                sig_reads = [Tsig[0], Tsig[1]] if (hd == 0 and qs < 2) else [Tsig[par]]
                def front(r):
                    hf = r // 64
                    ksl = slice(r * 128, (r + 1) * 128)
                    pS, TpS = psS.next()
                    S.op("pe", lambda h: h.matmul(pS[:, 0:512], lhsT=K1[:, ksl], rhs=Q1[:, qsl], start=True, stop=True), reads=[TK[hf], TQ] + sig_reads, writes=[TpS])
                    S.op("pe", lambda h: h.matmul(pS[:, 512:1024], lhsT=K2[:, ksl], rhs=Q2[:, qsl], start=True, stop=True), reads=[TK[hf], TQ] + sig_reads, writes=[TpS])
                    if r < 32:
                        dk = r - 4 * qs
                        if 0 <= dk < 4:
                            S.op("dve", lambda h: h.scalar_tensor_tensor(out=pS[:], in0=dmat[:, dk * 1024:(dk + 1) * 1024], scalar=-m, in1=pS[:], op0=ALU.mult, op1=ALU.add),
                                 reads=[TpS, Tdm], writes=[TpS])
                    pt, Tpt = PTr.next()
                    col = (hd * 8 + qs) * 128 + r
                    S.op("act", lambda h: h.activation(out=pt[:], in_=pS[:], func=AF.Exp, bias=btab[:, col:col + 1], scale=1.0),
                         reads=[TpS, Tbt], writes=[Tpt])
                    return pt, Tpt

                def back(r, pt, Tpt):
                    hf = r // 64
                    for a in range(8):
                        S.op("pe", lambda h, a=a: h.matmul(acc_ap(a, 0, 129), lhsT=pt[:, a * 128:(a + 1) * 128], rhs=Vs[:, r * 129:(r + 1) * 129], start=(r == 0 and a % 3 == 0), stop=(r == 127), skip_group_check=True),
                             reads=[Tpt, TV[hf]], writes=[Tacc])

                cur = front(0)
                for r in range(128):
                    nxt = front(r + 1) if r + 1 < 128 else None
                    back(r, *cur)
                    cur = nxt
                sb2, Ts = stg.next()
                for qt in range(4):
                    sm, Tsm = esm.next()
                    S.op("dve", lambda h, sm=sm, qt=qt: h.reciprocal(out=sm[:, 0:1], in_=acc_ap(qt, 128, 129)), reads=[Tacc], writes=[Tsm])
                    S.op("dve", lambda h, sm=sm, qt=qt: h.reciprocal(out=sm[:, 1:2], in_=acc_ap(4 + qt, 128, 129)), reads=[Tacc], writes=[Tsm])
                    S.op("dve", lambda h, sm=sm: h.tensor_tensor(out=sm[:, 2:3], in0=sm[:, 1:2], in1=lam_ap, op=ALU.mult), reads=[Tsm, Tlam], writes=[Tsm])
                    t2, Tt2 = t2r.next()
                    S.op("dve", lambda h, sm=sm, t2=t2, qt=qt: h.tensor_scalar(out=t2[:], in0=acc_ap(4 + qt, 0, 128), scalar1=sm[:, 2:3], scalar2=None, op0=ALU.mult), reads=[Tacc, Tsm], writes=[Tt2])
                    ob, To = o32.next()
                    S.op("dve", lambda h, sm=sm, t2=t2, ob=ob, qt=qt: h.scalar_tensor_tensor(out=ob[:], in0=acc_ap(qt, 0, 128), scalar=sm[:, 0:1], in1=t2[:], op0=ALU.mult, op1=ALU.subtract),
                         reads=[Tacc, Tsm, Tt2], writes=[To])
                    jb, Tj = junk.next()
                    S.op("act", lambda h, jb=jb, ob=ob, sm=sm: h.activation(out=jb[:], in_=ob[:], func=AF.Square, accum_out=sm[:, 3:4]), reads=[To], writes=[Tj, Tsm])
                    S.op("act", lambda h, sm=sm: h.activation(out=sm[:, 4:5], in_=sm[:, 3:4], func=AF.Sqrt, bias=eps[:, 0:1], scale=1.0 / 128), reads=[Tsm, Tg], writes=[Tsm])
                    S.op("dve", lambda h, sm=sm: h.reciprocal(out=sm[:, 5:6], in_=sm[:, 4:5]), reads=[Tsm], writes=[Tsm])
                    o16, To16 = ob16.next()
                    S.op("dve", lambda h, o16=o16, ob=ob, sm=sm: h.scalar_tensor_tensor(out=o16[:], in0=ob[:], scalar=sm[:, 5:6], in1=gbc[:], op0=ALU.mult, op1=ALU.mult),
                         reads=[To, Tsm, Tg], writes=[To16])
                    S.op("pe", lambda h, o16=o16, qt=qt: h.transpose(pst[:, qt * 128:(qt + 1) * 128], o16[:], ident[:]), reads=[To16, Tid], writes=[Tpst])
                S.op("dve", lambda h, sb2=sb2: h.tensor_copy(out=sb2[:], in_=pst[:]), reads=[Tpst], writes=[Ts])
                S.op("sp", lambda h, sb2=sb2: h.dma_start(out=BOT[hd * 128:(hd + 1) * 128, qsl], in_=sb2[:]), reads=[Ts], dma=True)

            for qs in range(nqs):
                do_qs(qs)
                if qs + 2 < nqs:
                    load_sig(qs + 2)

        for hd in range(heads):
            head(hd)
        S.emit(st)
    return nc


def prep_B(l, inp, A_out, c):
    b = c // 4
    c4 = c % 4
    cores = [b * 4 + j for j in range(4)]
    KTall = np.concatenate([np.asarray(A_out[j]["KT"]) for j in cores], axis=1)
    VVall = np.concatenate([np.asarray(A_out[j]["VV"]) for j in cores], axis=0)
    own = list(range(32 * c4, 32 * c4 + 32))
    order = own + [kb for kb in range(128) if kb not in own]
    order = np.asarray(order)
    sig = np.where(order < 32 * c4, 1.0, -1.0)
    sig[:32] = 0.0
    KAa = np.ascontiguousarray(KTall.reshape(NH, 2, 64, 128, 128)[:, :, :, order, :].reshape(NH, 2, 64, SEQ))
    sigt = np.zeros((8, 2, SEQ), dtype=NPBF)
    for qs in range(8):
        sq = sig.copy()
        sq[:4 * qs] = 1.0
        sq[4 * qs + 4:32] = -1.0
        sigt[qs, :, :] = np.repeat(sq, 128).astype(NPBF)[None, :]
    QT = np.asarray(A_out[c]["QT"]).reshape(NH, 2, 64, NT)
    QAa = np.zeros((NH, 2, 68, NT), dtype=NPBF)
    QAa[:, :, :64] = QT
    tq = np.arange(NT)
    i = tq % 512
    even = ((tq // 512) % 2 == 0)
    ihi = (i // 2) * 2
    ilo = i % 2
    for h in range(NH):
        m = 2.0 ** (-(h + 1))
        QAa[h, :, 64] = np.where(even, -m * ihi, 0.0).astype(NPBF)
        QAa[h, :, 65] = np.where(even, -m * ilo, 0.0).astype(NPBF)
        QAa[h, :, 66] = np.where(even, 0.0, -m * ihi).astype(NPBF)
        QAa[h, :, 67] = np.where(even, 0.0, -m * ilo).astype(NPBF)
    Vl = VVall.reshape(128, 128, NH, 128)[order]
    VHa = np.ones((NH, 128, 128, 129), dtype=NPBF)
    VHa[:, :, :, :128] = Vl.transpose(2, 1, 0, 3)
    p = np.arange(128, dtype=np.float64)
    bt = np.zeros((128, NH, 8, 128), dtype=np.float32)
    for qs in range(8):
        T0 = 4096 * c4 + 512 * qs
        rel = (order[None, :] * 128.0 + p[:, None]) - T0
        sgn = np.where(rel < 0, 1.0, -1.0)
        val = sgn * rel
        val[:, 4 * qs:4 * qs + 4] = 0.0
        for h in range(NH):
            bt[:, h, qs, :] = (2.0 ** (-(h + 1))) * val
    ii = np.arange(512, dtype=np.float32)
    dm = np.zeros((128, 4, 2, 512), dtype=np.float32)
    for dk in range(4):
        dm[:, dk, :, :] = np.abs(ii[None, :] - p[:, None] - 128 * dk)[:, None, :]
    negi = np.broadcast_to(np.tile(-ii, 2)[None, :], (128, 1024)).astype(np.float32)
    lamv = np.stack([np.asarray(inp[k][l]) for k in ("lam_q1", "lam_k1", "lam_q2", "lam_k2")]).astype(np.float32)
    return {"QA": QAa, "KA": KAa, "sigt": sigt, "VH": VHa.reshape(NH, 128, 128 * 129), "btab": bt.reshape(128, -1),
            "dmat": dm.reshape(128, -1), "negi": np.ascontiguousarray(negi), "lamv": lamv,
            "gdn": np.asarray(inp["diff_norm_g"][l]).reshape(1, 128), "ident": np.eye(128, dtype=np.float32)}


def build_C(final, ngroups=8, nexp=NEXP, upto=9):
    nc = bass.Bass("TRN2", target_bir_lowering=False)
    dr = lambda n, s, d, k: nc.dram_tensor(n, s, d, kind=k).ap()
    x = dr("x", [NT, D], F32, "ExternalInput")
    M0T = dr("M0T", [D, NT], F32, "ExternalInput")
    G1T = dr("G1T", [D, NT], F32, "ExternalInput")
    BOT = dr("BOT", [1024, NT], BF16, "ExternalInput")
    wbd = dr("wb", [16, 128, 8 * 128], F32, "ExternalInput")
    wod = dr("wo", [4, 128, 16 * 512], F32, "ExternalInput")
    g2 = dr("g2", [1, D], F32, "ExternalInput")
    fg = dr("fg", [1, D], F32, "ExternalInput")
    wrd = dr("wr", [128, 16 * 20], F32, "ExternalInput")
    brd = dr("br", [1, 20], F32, "ExternalInput")
    w1d = dr("w1t", [NEXP, 128, 16 * 512], F32, "ExternalInput")
    w3d = dr("w3t", [NEXP, 128, 16 * 512], F32, "ExternalInput")
    w2d = dr("w2t", [NEXP, 128, 4 * 2048], F32, "ExternalInput")
    ident_d = dr("ident", [128, 128], F32, "ExternalInput")
    XO = dr("XO", [NT, D], F32, "ExternalOutput")
    S = Sched(nc)
    AX = mybir.AxisListType.X
    with ExitStack() as st:
        sbt = lambda n, s, d: st.enter_context(nc.sbuf_tensor(n, s, d))
        ident = sbt("ident32", [128, 128], F32); Tid = T()
        g2bc = sbt("g2bc", [128, D], F32); fgbc = sbt("fgbc", [128, D], F32); Tg = T()
        wr = sbt("wrs", [128, 16, 20], F32); brbc = sbt("brbc", [128, 20], F32); Twr = T()
        eps = sbt("epsc", [128, 1], F32)
        x1 = [sbt(f"x1_{i}", [128, D], F32) for i in range(4)]; Tx1 = [T() for _ in range(4)]
        cw = [sbt(f"cw_{i}", [128, 16], F32) for i in range(4)]; Tcw = [T() for _ in range(4)]
        mT = sbt("mT", [128, 16, 512], BF16); TmT = T()
        bot = sbt("bot", [128, 8, 512], BF16); Tbot = T()
        h2T16 = sbt("h2T16", [128, 16, 512], BF16); Th16 = T()
        h2T32 = sbt("h2T32", [128, 16, 128], F32); Th32 = T()
        h2 = sbt("h2", [128, D], F32); Th2 = T()
        hidT = sbt("hidT", [128, 4, 512], BF16); Thid = T()
        wbig = Rot(st, nc, "wbig", [128, 8192], BF16, 4)
        wbb = Rot(st, nc, "wbb", [128, 8, 128], BF16, 2)
        gt = Rot(st, nc, "gt", [128, 512], F32, 2)
        mt = Rot(st, nc, "mt", [128, 512], F32, 2)
        sl = Rot(st, nc, "sl", [128, 512], F32, 2)
        junk = Rot(st, nc, "junkc", [128, D], BF16, 1)
        smr = Rot(st, nc, "smr", [128, 64], F32, 2)
        psm = Rot(st, nc, "psmc", [128, 512], F32, 5, psum=True)
        psr = Rot(st, nc, "psr", [128, 32], F32, 1, psum=True)

        S.op("sp", lambda h: h.dma_start(out=ident[:], in_=ident_d), writes=[Tid], dma=True)
        S.op("sp", lambda h: h.dma_start(out=g2bc[:], in_=g2.partition_broadcast(128)), writes=[Tg], dma=True)
        S.op("sp", lambda h: h.dma_start(out=fgbc[:], in_=fg.partition_broadcast(128)), writes=[Tg], dma=True)
        S.op("sp", lambda h: h.dma_start(out=wr[:].rearrange("p c n -> p (c n)"), in_=wrd), writes=[Twr], dma=True)
        S.op("sp", lambda h: h.dma_start(out=brbc[:], in_=brd.partition_broadcast(128)), writes=[Twr], dma=True)
        S.op("dve", lambda h: h.memset(eps[:], RMS_EPS), writes=[Tg])

        def rms(tt, dst_ap, gbc, Tdst_w):
            sm, Tsm = smr.next(); jb, Tj = junk.next()
            S.op("act", lambda h: h.activation(out=jb[:], in_=x1[tt][:], func=AF.Square, accum_out=sm[:, 0:1]), reads=[Tx1[tt]], writes=[Tj, Tsm])
            S.op("act", lambda h: h.activation(out=sm[:, 1:2], in_=sm[:, 0:1], func=AF.Sqrt, bias=eps[:, 0:1], scale=1.0 / D), reads=[Tsm, Tg], writes=[Tsm])
            S.op("dve", lambda h: h.reciprocal(out=sm[:, 2:3], in_=sm[:, 1:2]), reads=[Tsm], writes=[Tsm])
            S.op("dve", lambda h: h.scalar_tensor_tensor(out=dst_ap, in0=x1[tt][:], scalar=sm[:, 2:3], in1=gbc[:], op0=ALU.mult, op1=ALU.mult),
                 reads=[Tx1[tt], Tsm, Tg], writes=Tdst_w)

        def do_group(tg):
            cols = slice(tg * 512, (tg + 1) * 512)
            S.op("sp", lambda h: h.dma_start(out=bot[:], in_=BOT[:, cols].rearrange("(c p) t -> p c t", p=128)), writes=[Tbot], dma=True)
            for tt in range(4):
                S.op("sp", lambda h, tt=tt: h.dma_start(out=x1[tt][:], in_=x[tg * 512 + tt * 128:tg * 512 + (tt + 1) * 128, :]), writes=[Tx1[tt]], dma=True)
            for dc in range(16 if upto >= 1 else 0):
                wb_, Twb = wbb.next()
                S.op("pool", lambda h, wb_=wb_, dc=dc: h.dma_start(out=wb_[:].rearrange("p c n -> p (c n)"), in_=wbd[dc]), writes=[Twb], dma=True)
                pb, Tp = psm.next()
                _mm_group(S, pb[:], [(wb_[:, c, :], bot[:, c, :]) for c in range(8)], [Twb, Tbot], Tp)
                g_, Tg_ = gt.next(); m_, Tm_ = mt.next()
                S.op("sp", lambda h, g_=g_, dc=dc: h.dma_start(out=g_[:], in_=G1T[dc * 128:(dc + 1) * 128, cols]), writes=[Tg_], dma=True)
                S.op("sp", lambda h, m_=m_, dc=dc: h.dma_start(out=m_[:], in_=M0T[dc * 128:(dc + 1) * 128, cols]), writes=[Tm_], dma=True)
                S.op("dve", lambda h, g_=g_, pb=pb: h.tensor_tensor(out=g_[:], in0=pb[:], in1=g_[:], op=ALU.mult), reads=[Tp, Tg_], writes=[Tg_])
                S.op("pool", lambda h, g_=g_, m_=m_, dc=dc: h.tensor_tensor(out=mT[:, dc, :], in0=g_[:], in1=m_[:], op=ALU.add), reads=[Tg_, Tm_], writes=[TmT])
            for cgp in range(4 if upto >= 2 else 0):
                wo_, Two = wbig.next()
                S.op("pool", lambda h, wo_=wo_, cgp=cgp: h.dma_start(out=wo_[:], in_=wod[cgp]), writes=[Two], dma=True)
                for tt in range(4):
                    pb, Tp = psm.next()
                    _mm_group(S, pb[:], [(mT[:, c, tt * 128:(tt + 1) * 128], wo_[:, c * 512:(c + 1) * 512]) for c in range(16)], [Two, TmT], Tp)
                    S.op("dve", lambda h, pb=pb, tt=tt, cgp=cgp: h.tensor_tensor(out=x1[tt][:, cgp * 512:(cgp + 1) * 512], in0=pb[:], in1=x1[tt][:, cgp * 512:(cgp + 1) * 512], op=ALU.add),
                         reads=[Tp, Tx1[tt]], writes=[Tx1[tt]])
            for tt in range(4 if upto >= 3 else 0):
                rms(tt, h2[:], g2bc, [Th2])
                if upto < 4:
                    continue
                for c4 in range(4):
                    pb, Tp = psm.next()
                    for k in range(4):
                        c = c4 * 4 + k
                        S.op("pe", lambda h, pb=pb, c=c, k=k: h.matmul(pb[:, k * 128:(k + 1) * 128], lhsT=h2[:, c * 128:(c + 1) * 128], rhs=ident[:], start=True, stop=True), reads=[Th2, Tid], writes=[Tp])
                    src = pb[:].rearrange("p (k t) -> p k t", k=4)
                    S.op("act", lambda h, src=src, c4=c4: h.copy(out=h2T32[:, c4 * 4:(c4 + 1) * 4, :], in_=src), reads=[Tp], writes=[Th32])
                    S.op("dve", lambda h, src=src, c4=c4, tt=tt: h.tensor_copy(out=h2T16[:, c4 * 4:(c4 + 1) * 4, tt * 128:(tt + 1) * 128], in_=h2T32[:, c4 * 4:(c4 + 1) * 4, :]), reads=[Th32], writes=[Th16])
                if upto < 5:
                    continue
                pr, Tpr = psr.next()
                _mm_group(S, pr[:, 0:20], [(h2T32[:, c, :], wr[:, c, :]) for c in range(16)], [Th32, Twr], Tpr)
                sm, Tsm = smr.next()
                V = lambda fn, rd=(), sm=sm, Tsm=Tsm: S.op("dve", fn, reads=[Tsm] + list(rd), writes=[Tsm])
                S.op("dve", lambda h, sm=sm, pr=pr: h.tensor_tensor(out=sm[:, 0:20], in0=pr[:, 0:20], in1=brbc[:], op=ALU.add), reads=[Tpr, Twr], writes=[Tsm])
                V(lambda h, sm=sm: h.reduce_max(out=sm[:, 20:21], in_=sm[:, 0:4], axis=AX))
                V(lambda h, sm=sm: h.tensor_scalar(out=sm[:, 21:25], in0=sm[:, 0:4], scalar1=sm[:, 20:21], scalar2=None, op0=ALU.is_ge))
                V(lambda h, sm=sm: h.tensor_scalar(out=sm[:, 25:26], in0=sm[:, 20:21], scalar1=-1.0, scalar2=None, op0=ALU.mult))
                S.op("act", lambda h, sm=sm: h.activation(out=sm[:, 26:30], in_=sm[:, 0:4], func=AF.Exp, bias=sm[:, 25:26], scale=1.0, accum_out=sm[:, 30:31]), reads=[Tsm], writes=[Tsm])
                V(lambda h, sm=sm: h.reciprocal(out=sm[:, 31:32], in_=sm[:, 30:31]))
                V(lambda h, sm=sm: h.tensor_scalar(out=sm[:, 32:36], in0=sm[:, 4:8], scalar1=sm[:, 21:22], scalar2=None, op0=ALU.mult))
                for g in range(1, 4):
                    V(lambda h, sm=sm, g=g: h.scalar_tensor_tensor(out=sm[:, 32:36], in0=sm[:, 4 + 4 * g:8 + 4 * g], scalar=sm[:, 21 + g:22 + g], in1=sm[:, 32:36], op0=ALU.mult, op1=ALU.add))
                V(lambda h, sm=sm: h.reduce_max(out=sm[:, 36:37], in_=sm[:, 32:36], axis=AX))
                V(lambda h, sm=sm: h.tensor_scalar(out=sm[:, 37:41], in0=sm[:, 32:36], scalar1=sm[:, 36:37], scalar2=None, op0=ALU.is_ge))
                V(lambda h, sm=sm: h.scalar_tensor_tensor(out=sm[:, 41:45], in0=sm[:, 37:41], scalar=-1e30, in1=sm[:, 32:36], op0=ALU.mult, op1=ALU.add))
                V(lambda h, sm=sm: h.reduce_max(out=sm[:, 45:46], in_=sm[:, 41:45], axis=AX))
                V(lambda h, sm=sm: h.tensor_scalar(out=sm[:, 46:50], in0=sm[:, 41:45], scalar1=sm[:, 45:46], scalar2=None, op0=ALU.is_ge))
                V(lambda h, sm=sm: h.tensor_tensor(out=sm[:, 50:51], in0=sm[:, 45:46], in1=sm[:, 36:37], op=ALU.subtract))
                S.op("act", lambda h, sm=sm: h.activation(out=sm[:, 51:52], in_=sm[:, 50:51], func=AF.Exp), reads=[Tsm], writes=[Tsm])
                V(lambda h, sm=sm: h.tensor_scalar(out=sm[:, 52:53], in0=sm[:, 51:52], scalar1=1.0, scalar2=None, op0=ALU.add))
                V(lambda h, sm=sm: h.reciprocal(out=sm[:, 53:54], in_=sm[:, 52:53]))
                V(lambda h, sm=sm: h.tensor_tensor(out=sm[:, 54:55], in0=sm[:, 53:54], in1=sm[:, 31:32], op=ALU.mult))
                V(lambda h, sm=sm: h.tensor_tensor(out=sm[:, 55:56], in0=sm[:, 31:32], in1=sm[:, 54:55], op=ALU.subtract))
                V(lambda h, sm=sm: h.tensor_scalar(out=sm[:, 56:60], in0=sm[:, 37:41], scalar1=sm[:, 54:55], scalar2=None, op0=ALU.mult))
                V(lambda h, sm=sm: h.scalar_tensor_tensor(out=sm[:, 56:60], in0=sm[:, 46:50], scalar=sm[:, 55:56], in1=sm[:, 56:60], op0=ALU.mult, op1=ALU.add))
                for g in range(4):
                    S.op("dve", lambda h, sm=sm, g=g, tt=tt: h.tensor_scalar(out=cw[tt][:, 4 * g:4 * g + 4], in0=sm[:, 56:60], scalar1=sm[:, 21 + g:22 + g], scalar2=None, op0=ALU.mult),
                         reads=[Tsm], writes=[Tcw[tt]])
            for e in range(nexp):
                w1_, Tw1 = wbig.next()
                S.op("pool", lambda h, w1_=w1_, e=e: h.dma_start(out=w1_[:], in_=w1d[e]), writes=[Tw1], dma=True)
                w3_, Tw3 = wbig.next()
                S.op("pool", lambda h, w3_=w3_, e=e: h.dma_start(out=w3_[:], in_=w3d[e]), writes=[Tw3], dma=True)
                w2_, Tw2 = wbig.next()
                S.op("pool", lambda h, w2_=w2_, e=e: h.dma_start(out=w2_[:], in_=w2d[e]), writes=[Tw2], dma=True)
                for fc in range(4):
                    p1, Tp1 = psm.next()
                    _mm_group(S, p1[:], [(w1_[:, c * 512 + fc * 128:c * 512 + (fc + 1) * 128], h2T16[:, c, :]) for c in range(16)], [Tw1, Th16], Tp1)
                    p3, Tp3 = psm.next()
                    _mm_group(S, p3[:], [(w3_[:, c * 512 + fc * 128:c * 512 + (fc + 1) * 128], h2T16[:, c, :]) for c in range(16)], [Tw3, Th16], Tp3)
                    s_, Ts_ = sl.next()
                    S.op("act", lambda h, s_=s_, p1=p1: h.activation(out=s_[:], in_=p1[:], func=AF.Silu), reads=[Tp1], writes=[Ts_])
                    S.op("dve", lambda h, s_=s_, p3=p3, fc=fc: h.tensor_tensor(out=hidT[:, fc, :], in0=p3[:], in1=s_[:], op=ALU.mult), reads=[Tp3, Ts_], writes=[Thid])
                for tt in range(4):
                    for cgp in range(4):
                        py, Tpy = psm.next()
                        _mm_group(S, py[:], [(hidT[:, fc, tt * 128:(tt + 1) * 128], w2_[:, fc * 2048 + cgp * 512:fc * 2048 + (cgp + 1) * 512]) for fc in range(4)], [Tw2, Thid], Tpy)
                        S.op("dve", lambda h, py=py, tt=tt, cgp=cgp, e=e: h.scalar_tensor_tensor(out=x1[tt][:, cgp * 512:(cgp + 1) * 512], in0=py[:], scalar=cw[tt][:, e:e + 1], in1=x1[tt][:, cgp * 512:(cgp + 1) * 512], op0=ALU.mult, op1=ALU.add),
                             reads=[Tpy, Tcw[tt], Tx1[tt]], writes=[Tx1[tt]])
            for tt in range(4):
                rows = slice(tg * 512 + tt * 128, tg * 512 + (tt + 1) * 128)
                if final:
                    rms(tt, h2[:], fgbc, [Th2])
                    S.op("sp", lambda h, rows=rows: h.dma_start(out=XO[rows, :], in_=h2[:]), reads=[Th2], dma=True)
                else:
                    S.op("sp", lambda h, rows=rows, tt=tt: h.dma_start(out=XO[rows, :], in_=x1[tt][:]), reads=[Tx1[tt]], dma=True)

        for tg in range(ngroups):
            do_group(tg)
        S.emit(st)
    return nc


def prep_C(l, inp):
    wrr = np.concatenate([np.asarray(inp["router_g_w"][l]), np.asarray(inp["router_e_w"][l])], axis=1)
    return {
        "wb": _tile_km(np.asarray(inp["w_proj_b"][l]), 128),
        "wo": _tile_km(np.asarray(inp["w_out"][l]), 512),
        "g2": np.asarray(inp["norm2_g"][l]).reshape(1, D),
        "fg": np.asarray(inp["final_g"]).reshape(1, D),
        "wr": np.ascontiguousarray(wrr.reshape(16, 128, 20).transpose(1, 0, 2)).reshape(128, 320),
        "br": np.concatenate([np.asarray(inp["router_g_b"][l]), np.asarray(inp["router_e_b"][l])]).reshape(1, 20),
        "w1t": np.stack([_tile_km(np.asarray(inp["w1"][l][e]), 512)[0] for e in range(NEXP)]),
        "w3t": np.stack([_tile_km(np.asarray(inp["w3"][l][e]), 512)[0] for e in range(NEXP)]),
        "w2t": np.stack([_tile_km(np.asarray(inp["w2"][l][e]), 2048)[0] for e in range(NEXP)]),
        "ident": np.eye(128, dtype=np.float32),
    }


_PROG = {}


def _prog(key, fn):
    if key not in _PROG:
        _PROG[key] = fn()
    return _PROG[key]


def kernel(**inp):
    cores = list(range(NCORE))
    x = np.ascontiguousarray(np.asarray(inp["x"], dtype=np.float32).reshape(NCORE, NT, D))
    xs = [x[c] for c in cores]
    depth = inp["w_in"].shape[0]
    for l in range(depth):
        lam_init = 0.8 - 0.6 * math.exp(-0.3 * l)
        pa = prep_A(l, inp)
        ra = run_bass_kernel_spmd(_prog("A", build_A), [dict(pa, x=xs[c]) for c in cores], core_ids=cores).results
        del pa
        rb = run_bass_kernel_spmd(_prog(("B", l), lambda: build_B(lam_init)), [prep_B(l, inp, ra, c) for c in cores], core_ids=cores).results
        pc = prep_C(l, inp)
        final = (l == depth - 1)
        rc = run_bass_kernel_spmd(_prog(("C", final), lambda: build_C(final)),
                                  [dict(pc, x=xs[c], M0T=ra[c]["M0T"], G1T=ra[c]["G1T"], BOT=rb[c]["BOT"]) for c in cores], core_ids=cores).results
        xs = [np.asarray(rc[c]["XO"]) for c in cores]
        del pc, ra, rb, rc
    return np.stack(xs).reshape(inp["x"].shape).astype(np.float32)
```
